# Optimizing a Trainium2 kernel written in Bass

```python
import math
import jax, jax.numpy as jnp
from jax import lax
import numpy as np

D_MODEL = 1024
BATCH = 2
SEQ = 8192
DEPTH = 1

D_CONV = D_MODEL
CONV_WIDTH = 3
MLA_HEADS = 8
QK_NOPE = 128
QK_ROPE = 64
V_HEAD = 128
Q_LORA = 256
KV_LORA = 128
ROPE_THETA = 10000.0
Q_BLOCK = 128
MEM_LEN = 256
MEM_HEADS = 4
MEM_HEAD_DIM = D_MODEL // MEM_HEADS
N_EXPERTS = 16
EXPERT_FF = 1024
CAPACITY_FACTOR = 2
EPS = 1e-6
IN_COLS = 3 * D_CONV + Q_LORA + KV_LORA + QK_ROPE + 2 * D_MODEL

kernel_name = "hybrid_conv_mla_ec_moe_encoder"


def rmsnorm(x, g):
    xf = x.astype(jnp.float32)
    y = xf * lax.rsqrt(jnp.mean(xf * xf, axis=-1, keepdims=True) + EPS)
    return (y * g.astype(jnp.float32)).astype(x.dtype)


def rotary_tables(seq):
    inv = 1.0 / (ROPE_THETA ** (jnp.arange(0, QK_ROPE, 2, dtype=jnp.float32) / QK_ROPE))
    ang = jnp.arange(seq, dtype=jnp.float32)[:, None] * inv[None, :]
    return jnp.cos(ang), jnp.sin(ang)


def apply_rope(v, cos, sin):
    vf = v.astype(jnp.float32)
    v1, v2 = jnp.split(vf, 2, axis=-1)
    out = jnp.concatenate([v1 * cos - v2 * sin, v1 * sin + v2 * cos], axis=-1)
    return out.astype(v.dtype)


def short_conv(v, w, b):
    y = lax.conv_general_dilated(
        v, w[:, None, :].astype(v.dtype), window_strides=(1,), padding=((1, 1),),
        dimension_numbers=("NWC", "WIO", "NWC"), feature_group_count=v.shape[-1])
    return y + b.astype(v.dtype)


def mla_attention(q_nope, q_rope, k_nope, k_rope, v):
    b, s, h, _ = q_nope.shape
    nb = s // Q_BLOCK
    scale = (QK_NOPE + QK_ROPE) ** -0.5
    qn = q_nope.reshape(b, nb, Q_BLOCK, h, QK_NOPE).transpose(1, 0, 2, 3, 4)
    qr = q_rope.reshape(b, nb, Q_BLOCK, h, QK_ROPE).transpose(1, 0, 2, 3, 4)

    def block(args):
        qn_b, qr_b = args
        sc = (jnp.einsum("bqhd,bkhd->bhqk", qn_b, k_nope)
              + jnp.einsum("bqhr,bkr->bhqk", qr_b, k_rope))
        p = jax.nn.softmax(sc.astype(jnp.float32) * scale, axis=-1).astype(v.dtype)
        return jnp.einsum("bhqk,bkhd->bqhd", p, v)

    o = lax.map(block, (qn, qr))
    return o.transpose(1, 0, 2, 3, 4).reshape(b, s, h * V_HEAD)


def memory_cross_attention(h, m, w_q, w_kv, w_o):
    b, s, _ = h.shape
    ml = m.shape[1]
    q = (h @ w_q).reshape(b, s, MEM_HEADS, MEM_HEAD_DIM)
    kv = (m @ w_kv).reshape(b, ml, 2, MEM_HEADS, MEM_HEAD_DIM)
    k, v = kv[:, :, 0], kv[:, :, 1]
    sc = jnp.einsum("bqhd,bkhd->bhqk", q, k).astype(jnp.float32) * (MEM_HEAD_DIM ** -0.5)
    p = jax.nn.softmax(sc, axis=-1).astype(v.dtype)
    o = jnp.einsum("bhqk,bkhd->bqhd", p, v).reshape(b, s, MEM_HEADS * MEM_HEAD_DIM)
    return o @ w_o


def expert_choice_moe(h, w_router, w_gate, w_up, w_down):
    b, s, d = h.shape
    cap = max(1, CAPACITY_FACTOR * s // N_EXPERTS)
    aff = jax.nn.softmax((h @ w_router).astype(jnp.float32), axis=-1)
    gates, idx = lax.top_k(jnp.swapaxes(aff, 1, 2), cap)
    xg = jax.vmap(lambda hb, ib: hb[ib])(h, idx)
    a = jax.nn.silu(jnp.einsum("becd,edf->becf", xg, w_gate)) * jnp.einsum("becd,edf->becf", xg, w_up)
    yo = jnp.einsum("becf,efd->becd", a, w_down) * gates[..., None].astype(h.dtype)
    return jax.vmap(lambda ib, vb: jnp.zeros((s, d), vb.dtype).at[ib.reshape(-1)].add(vb.reshape(-1, d)))(idx, yo)


def setup_inputs(seed: int = 0) -> dict:
    key = jax.random.key(seed)
    ks = jax.random.split(key, 32)

    def w(k, shape, fan_in):
        return jax.random.normal(k, shape, jnp.float32) * fan_in ** -0.5

    def g(k, shape):
        return 1.0 + 0.02 * jax.random.normal(k, shape, jnp.float32)

    def bias(k, shape):
        return 0.02 * jax.random.normal(k, shape, jnp.float32)

    L = DEPTH
    return {
        "x": jax.random.normal(ks[0], (BATCH, SEQ, D_MODEL), jnp.float32),
        "mem": jax.random.normal(ks[1], (BATCH, MEM_LEN, D_MODEL), jnp.float32),
        "norm_mix_g": g(ks[2], (L, D_MODEL)),
        "w_in": w(ks[3], (L, D_MODEL, IN_COLS), D_MODEL),
        "conv_w": w(ks[4], (L, CONV_WIDTH, D_CONV), CONV_WIDTH),
        "conv_b": bias(ks[5], (L, D_CONV)),
        "w_conv_out": w(ks[6], (L, D_CONV, D_MODEL), D_CONV),
        "q_norm_g": g(ks[7], (L, Q_LORA)),
        "w_uq": w(ks[8], (L, Q_LORA, MLA_HEADS * (QK_NOPE + QK_ROPE)), Q_LORA),
        "kv_norm_g": g(ks[9], (L, KV_LORA)),
        "w_ukv": w(ks[10], (L, KV_LORA, MLA_HEADS * (QK_NOPE + V_HEAD)), KV_LORA),
        "w_mla_out": w(ks[11], (L, MLA_HEADS * V_HEAD, D_MODEL), MLA_HEADS * V_HEAD),
        "b_gate": bias(ks[12], (L, 2 * D_MODEL)),
        "w_mix_out": w(ks[13], (L, D_MODEL, D_MODEL), D_MODEL),
        "norm_mem_g": g(ks[14], (L, D_MODEL)),
        "norm_memkv_g": g(ks[15], (L, D_MODEL)),
        "w_mem_q": w(ks[16], (L, D_MODEL, MEM_HEADS * MEM_HEAD_DIM), D_MODEL),
        "w_mem_kv": w(ks[17], (L, D_MODEL, 2 * MEM_HEADS * MEM_HEAD_DIM), D_MODEL),
        "w_mem_out": w(ks[18], (L, MEM_HEADS * MEM_HEAD_DIM, D_MODEL), MEM_HEADS * MEM_HEAD_DIM),
        "norm_moe_g": g(ks[19], (L, D_MODEL)),
        "w_router": w(ks[20], (L, D_MODEL, N_EXPERTS), D_MODEL),
        "w_exp_gate": w(ks[21], (L, N_EXPERTS, D_MODEL, EXPERT_FF), D_MODEL),
        "w_exp_up": w(ks[22], (L, N_EXPERTS, D_MODEL, EXPERT_FF), D_MODEL),
        "w_exp_down": w(ks[23], (L, N_EXPERTS, EXPERT_FF, D_MODEL), EXPERT_FF),
        "norm_final_g": g(ks[24], (D_MODEL,)),
    }


def reference(x, mem, norm_mix_g, w_in, conv_w, conv_b, w_conv_out, q_norm_g, w_uq, kv_norm_g,
              w_ukv, w_mla_out, b_gate, w_mix_out, norm_mem_g, norm_memkv_g, w_mem_q, w_mem_kv,
              w_mem_out, norm_moe_g, w_router, w_exp_gate, w_exp_up, w_exp_down, norm_final_g):
    b, s, _ = x.shape
    cos, sin = rotary_tables(s)
    splits = list(np.cumsum([D_CONV, D_CONV, D_CONV, Q_LORA, KV_LORA, QK_ROPE]))
    for l in range(DEPTH):
        h = rmsnorm(x, norm_mix_g[l])
        u = h @ w_in[l]
        xc, gb, gc, cq, ckv, kr, glog = jnp.split(u, splits, axis=-1)
        y_conv = (gb * short_conv(gc * xc, conv_w[l], conv_b[l])) @ w_conv_out[l]
        q = (rmsnorm(cq, q_norm_g[l]) @ w_uq[l]).reshape(b, s, MLA_HEADS, QK_NOPE + QK_ROPE)
        q_nope, q_rope = q[..., :QK_NOPE], apply_rope(q[..., QK_NOPE:], cos[None, :, None, :], sin[None, :, None, :])
        kv = (rmsnorm(ckv, kv_norm_g[l]) @ w_ukv[l]).reshape(b, s, MLA_HEADS, QK_NOPE + V_HEAD)
        k_nope, v = kv[..., :QK_NOPE], kv[..., QK_NOPE:]
        k_rope = apply_rope(kr, cos[None], sin[None])
        y_mla = mla_attention(q_nope, q_rope, k_nope, k_rope, v) @ w_mla_out[l]
        gates = jax.nn.sigmoid((glog + b_gate[l]).astype(jnp.float32)).astype(x.dtype)
        g_conv, g_mla = gates[..., :D_MODEL], gates[..., D_MODEL:]
        x = x + (g_conv * y_conv + g_mla * y_mla) @ w_mix_out[l]
        x = x + memory_cross_attention(rmsnorm(x, norm_mem_g[l]), rmsnorm(mem, norm_memkv_g[l]),
                                       w_mem_q[l], w_mem_kv[l], w_mem_out[l])
        x = x + expert_choice_moe(rmsnorm(x, norm_moe_g[l]), w_router[l], w_exp_gate[l],
                                  w_exp_up[l], w_exp_down[l])
    return rmsnorm(x, norm_final_g)
```

```python
import os
import numpy as np
from contextlib import ExitStack
import concourse.bass as bass
import concourse.mybir as mybir
from concourse.bass_utils import run_bass_kernel_spmd

F32 = mybir.dt.float32
BF16 = mybir.dt.bfloat16
I32 = mybir.dt.int32
ALU = mybir.AluOpType
AF = mybir.ActivationFunctionType
AX = mybir.AxisListType

D = 1024
S = 8192
TOK = 2048
EPS = 1e-6
NCST = 83


class Sched:
    ENG = ("pe", "act", "dve", "pool", "sp")

    def __init__(self, nc, es):
        self.nc = nc
        self.es = es
        self.ops = {e: [] for e in self.ENG}
        self.sem = {e: es.enter_context(nc.semaphore("s_" + e)) for e in self.ENG}
        self.cnt = {e: 0 for e in self.ENG}
        self.waited = {}
        self.lastw = {}
        self.readers = {}

    def _need(self, eng, tok, waits):
        if tok is None:
            return
        key, val = tok
        if key == eng and eng == "pe":
            return
        if self.waited.get((eng, key), 0) >= val:
            return
        waits[key] = max(waits.get(key, 0), val)

    def _deps(self, eng, reads, writes):
        waits = {}
        for k in reads:
            self._need(eng, self.lastw.get(k), waits)
        for k in writes:
            self._need(eng, self.lastw.get(k), waits)
            for tok in self.readers.get(k, ()):
                self._need(eng, tok, waits)
        for key, val in waits.items():
            self.waited[(eng, key)] = val
            self.ops[eng].append(("w", self.sem[key], val))

    def _commit(self, tok, reads, writes):
        for k in reads:
            self.readers.setdefault(k, []).append(tok)
        for k in writes:
            self.lastw[k] = tok
            self.readers[k] = []

    def op(self, eng, fn, reads=(), writes=(), inc=True):
        self._deps(eng, reads, writes)
        if inc:
            self.cnt[eng] += 1
            tok = (eng, self.cnt[eng])
            self.ops[eng].append(("i", fn, self.sem[eng], 1))
        else:
            tok = (eng, self.cnt[eng] + 1)
            self.ops[eng].append(("n", fn))
        self._commit(tok, reads, writes)
        return tok

    def dma(self, eng, fn, dkey, reads=(), writes=(), inc=16):
        if dkey not in self.sem:
            self.sem[dkey] = self.es.enter_context(self.nc.semaphore("d_" + dkey))
            self.cnt[dkey] = 0
        self._deps(eng, reads, writes)
        self.cnt[dkey] += inc
        tok = (dkey, self.cnt[dkey])
        self.ops[eng].append(("i", fn, self.sem[dkey], inc))
        self._commit(tok, reads, writes)
        return tok

    def barrier(self, skip=()):
        for eng in self.ENG:
            for key, val in self.cnt.items():
                if key == eng or val == 0 or key in skip:
                    continue
                if self.waited.get((eng, key), 0) >= val:
                    continue
                self.waited[(eng, key)] = val
                self.ops[eng].append(("w", self.sem[key], val))

    def emit(self):
        nc = self.nc
        ops = self.ops
        self.ops = {e: [] for e in self.ENG}
        with nc.Block() as block:
            def run(engname):
                def body(e):
                    for o in ops[engname]:
                        if o[0] == "w":
                            e.wait_ge(o[1], o[2])
                        elif o[0] == "n":
                            o[1](e)
                        else:
                            o[1](e).then_inc(o[2], o[3])
                return body
            block.tensor(run("pe"))
            block.scalar(run("act"))
            block.vector(run("dve"))
            block.gpsimd(run("pool"))
            block.sync(run("sp"))


def bc(ap2, n):
    return ap2.unsqueeze(2).to_broadcast([ap2.shape[0], ap2.shape[1], n])


def build(debug=False):
    nc = bass.Bass("TRN2", target_bir_lowering=False)
    di = {}

    def inp(name, shape, dt=F32):
        di[name] = nc.dram_tensor(name, shape, dt, kind="ExternalInput").ap()
        return di[name]

    xTb = inp("xTb", [D, S])
    xown = inp("xown", [D, TOK + 2])
    memT = inp("memT", [D, 256])
    w_in = inp("w_in", [D, 5568])
    w_krsw = inp("w_krsw", [D, 64])
    w_conv_out = inp("w_conv_out", [D, D])
    w_uq = inp("w_uq", [256, 1536])
    w_uqs = inp("w_uqs", [256, 512])
    w_ukT = inp("w_ukT", [128, 8, 128])
    w_ukv = inp("w_ukv", [128, 2048])
    w_mla_out = inp("w_mla_out", [D, D])
    w_mix_out = inp("w_mix_out", [D, D])
    w_mem_q = inp("w_mem_q", [D, D])
    w_mem_kv = inp("w_mem_kv", [D, 2048])
    w_mem_out = inp("w_mem_out", [D, D])
    w_router = inp("w_router", [D, 16])
    w_eg = inp("w_eg", [4, D, D])
    w_eu = inp("w_eu", [4, D, D])
    w_ed = inp("w_ed", [4, D, D])
    cst_d = inp("cst", [128, NCST])
    gfin_d = inp("gfin", [128, D])
    ident_d = inp("ident", [128, 128])
    tri_d = inp("tri", [128, 128])
    iot_d = inp("iot", [128, 16])
    iots_d = inp("iots", [128, 1024])
    ropeb = inp("ropeb", [64, 2, S])
    ropeo = inp("ropeo", [64, 2, TOK])
    myrows_d = inp("myrows", [128, 4], I32)
    gsel_d = inp("gsel", [128, 4, 16])
    out = nc.dram_tensor("out", [TOK, D], F32, kind="ExternalOutput").ap()
    dbg = {}
    if debug:
        dbg["x1"] = nc.dram_tensor("dbg_x1", [D, TOK], F32, kind="ExternalOutput").ap()
        dbg["x2"] = nc.dram_tensor("dbg_x2", [TOK, D], F32, kind="ExternalOutput").ap()
        dbg["R"] = nc.dram_tensor("dbg_R", [TOK, D], F32, kind="ExternalOutput").ap()
        dbg["idx"] = nc.dram_tensor("dbg_idx", [128, 32], I32, kind="ExternalOutput").ap()
        dbg["thr"] = nc.dram_tensor("dbg_thr", [128, 4], F32, kind="ExternalOutput").ap()

    Hloc = nc.dram_tensor("Hloc", [TOK, 1024], BF16).ap()
    Hall = nc.dram_tensor("Hall", [4 * TOK, 1024], BF16).ap()
    Gloc = nc.dram_tensor("Gloc", [TOK, 16], F32).ap()
    Gall = nc.dram_tensor("Gall", [4 * TOK, 16], F32).ap()
    Aloc = nc.dram_tensor("Aloc", [16, TOK], F32).ap()
    Aall = nc.dram_tensor("Aall", [64, TOK], F32).ap()
    X2loc = nc.dram_tensor("X2loc", [TOK, D], F32).ap()
    Dd = nc.dram_tensor("Dd", [4 * TOK, D], F32).ap()
    Rr = nc.dram_tensor("Rr", [TOK, D], F32).ap()
    GROUPS = [[0, 1, 2, 3], [4, 5, 6, 7]]

    with ExitStack() as es:
        sch = Sched(nc, es)
        op, dma = sch.op, sch.dma

        def sbt(stack, name, shape, dt):
            return stack.enter_context(nc.sbuf_tensor("sb_" + name, shape, dt))

        pbig = es.enter_context(nc.psum_tensor("pbig", [128, 8, 512], F32))
        pb = [pbig[:, i, :] for i in range(8)]
        PB = ["pb%d" % i for i in range(8)]

        cst = sbt(es, "cst", [128, NCST], F32)
        onesb = sbt(es, "onesb", [128, 128], BF16)
        identb = sbt(es, "identb", [128, 128], BF16)
        identf = sbt(es, "identf", [128, 128], F32)
        rstd_own = sbt(es, "rstd_own", [128, TOK + 2], F32)
        zt = sbt(es, "zt", [128, 2048], F32)
        dma("sp", lambda e: e.dma_start(out=cst[:], in_=cst_d), "c0", writes=["cst"])
        dma("sp", lambda e: e.dma_start(out=identf[:], in_=ident_d), "c1", writes=["identf"])
        dma("pool", lambda e: e.dma_start(out=identb[:], in_=ident_d), "c2", writes=["identb"])
        op("dve", lambda e: e.memset(onesb[:], 1.0), writes=["onesb"])
        GMIX = cst[:, 0:8]
        CW = [cst[:, 8:16], cst[:, 16:24], cst[:, 24:32]]
        CB = cst[:, 32:40]
        GQ = cst[:, 40:42]
        GKV = cst[:, 42:43]
        BG = cst[:, 43:59]
        GMEM = cst[:, 59:67]
        GMKV = cst[:, 67:75]
        GMOE = cst[:, 75:83]

        def load_w(stack_t, src_ap, key, fold=False, eng="pool"):
            dma("pool", lambda e: e.dma_start(out=stack_t[:], in_=src_ap.rearrange("(k p) c -> p k c", p=128)), "w_" + key, writes=[key])
            if fold:
                n = stack_t.shape[2]
                op(eng, lambda e: e.tensor_tensor(out=stack_t[:], in0=stack_t[:], in1=bc(GMIX, n), op=ALU.mult), reads=[key, "cst"], writes=[key])

        def rms_rstd(xsq_ap_fn, nk, n, out_ap, keys_r, key_w, inv_dim, bank=0, tmpkey="rt"):
            for k in range(nk):
                op("pe", lambda e, k=k: e.matmul(pb[bank][:, 0:n], lhsT=onesb[:], rhs=xsq_ap_fn(k), start=(k == 0), stop=(k == nk - 1)),
                   reads=["onesb"] + keys_r, writes=[PB[bank]], inc=(k == nk - 1))
            op("act", lambda e: e.activation(out=out_ap, in_=pb[bank][:, 0:n], func=AF.Ln, bias=EPS, scale=inv_dim), reads=[PB[bank]], writes=[key_w])
            op("act", lambda e: e.activation(out=out_ap, in_=out_ap, func=AF.Exp, scale=-0.5), reads=[key_w], writes=[key_w])

        def mmg(bank, n, pairs, reads, m=128, col0=0):
            L = len(pairs)
            for i, (l, r) in enumerate(pairs):
                op("pe", lambda e, i=i, l=l, r=r: e.matmul(pb[bank][0:m, col0:col0 + n], lhsT=l, rhs=r, start=(i == 0), stop=(i == L - 1)),
                   reads=reads, writes=[PB[bank]], inc=(i == L - 1))

        TOP = nc._sbuf_addr_for_side("right")
        attn = nc.alloc_sbuf_tensor_at("sb_attn", [128, 8, TOK], BF16, offset=TOP - 32768)
        mixed = nc.alloc_sbuf_tensor_at("sb_mixed", [128, 8, TOK], BF16, offset=TOP - 98304)
        x1 = nc.alloc_sbuf_tensor_at("sb_x1", [128, 8, TOK], F32, offset=TOP - 65536)
        with ExitStack() as p1:
            Kn = sbt(p1, "Kn", [128, S], BF16)
            Kr = sbt(p1, "Kr", [128, S], BF16)
            V = sbt(p1, "V", [128, 64, 128], BF16)
            w1 = sbt(p1, "w1", [128, 8, 256], BF16)
            wcq = sbt(p1, "wcq", [128, 8, 256], BF16)
            wuq = sbt(p1, "wuq", [128, 2, 1536], BF16)
            wuqs = sbt(p1, "wuqs", [128, 2, 512], BF16)
            wukT = sbt(p1, "wukT", [128, 8, 128], BF16)
            wuv = sbt(p1, "wuv", [128, 8, 128], BF16)
            xbt = [sbt(p1, "xbt%d" % i, [128, 8, 512], BF16) for i in range(2)]
            xsq = sbt(p1, "xsq", [128, 8, 512], BF16)
            cs = [sbt(p1, "cs%d" % i, [64, 2, 512], F32) for i in range(2)]
            rstd = sbt(p1, "rstd", [128, 512], F32)
            ckv32 = sbt(p1, "ckv32", [128, 512], F32)
            sq2 = sbt(p1, "sq2", [128, 2, 512], BF16)
            r2 = sbt(p1, "r2", [128, 512], F32)
            t1 = sbt(p1, "t1", [64, 512], F32)
            t2 = sbt(p1, "t2", [64, 512], F32)
            cq32 = sbt(p1, "cq32", [128, 2, 512], F32)
            cqn = sbt(p1, "cqn", [128, 2, 512], BF16)
            qn = sbt(p1, "qn", [128, 512], BF16)
            qabs = sbt(p1, "qabs", [128, 8, 512], BF16)
            qrope = sbt(p1, "qrope", [128, 8, 512], BF16)
            Pp = [sbt(p1, "Pp%d" % i, [128, 2, 512], BF16) for i in range(3)]
            Ps2 = [sbt(p1, "Ps2_%d" % i, [128, 512], BF16) for i in range(2)]
            rD = sbt(p1, "rD", [128, 512], F32)
            pc = sbt(p1, "pc", [128, 512], BF16)

            op("pool", lambda e: e.memset(Kr[64:128, :], 0.0), writes=["Krz"])
            op("pool", lambda e: e.memset(qrope[64:128, :, :], 0.0), writes=["qropez"])
            dma("pool", lambda e: e.dma_start(out=w1[:, :, 0:192], in_=w_in[:, 3328:3520].rearrange("(k p) c -> p k c", p=128)), "w_w1a", writes=["w1"])
            dma("pool", lambda e: e.dma_start(out=w1[:, :, 192:256], in_=w_krsw.rearrange("(k p) c -> p k c", p=128)), "w_w1b", writes=["w1"])
            op("pool", lambda e: e.tensor_tensor(out=w1[:], in0=w1[:], in1=bc(GMIX, 256), op=ALU.mult), reads=["w1", "cst"], writes=["w1"])
            load_w(wcq, w_in[:, 3072:3328], "wcq", fold=True)
            dma("pool", lambda e: e.dma_start(out=wuq[:], in_=w_uq.rearrange("(k p) c -> p k c", p=128)), "w_wuq", writes=["wuq"])
            dma("pool", lambda e: e.dma_start(out=wuqs[:], in_=w_uqs.rearrange("(k p) c -> p k c", p=128)), "w_wuqs", writes=["wuqs"])
            dma("pool", lambda e: e.dma_start(out=wukT[:], in_=w_ukT), "w_wukT", writes=["wukT"])
            dma("pool", lambda e: e.dma_start(out=wuv[:], in_=w_ukv.rearrange("l (h c) -> l h c", c=256)[:, :, 128:256]), "w_wuv", writes=["wuv"])

            def load_tile(i, src, col0, rope_src, rcol0):
                b = i % 2
                dma("pool", lambda e: e.dma_start(out=xbt[b][:], in_=src[:, col0:col0 + 512].rearrange("(k p) t -> p k t", p=128)), "xbt%d" % b, writes=["xbt%d" % b])
                dma("sp", lambda e: e.dma_start(out=cs[b][:], in_=rope_src[:, :, rcol0:rcol0 + 512]), "cs%d" % b, writes=["cs%d" % b])

            def tile_rstd(b, dst_ap, dst_key):
                op("act", lambda e: e.activation(out=xsq[:], in_=xbt[b][:], func=AF.Square), reads=["xbt%d" % b], writes=["xsq"])
                rms_rstd(lambda k: xsq[:, k, :], 8, 512, dst_ap, ["xsq"], dst_key, 1.0 / D, bank=0)

            def rope_to(dst_ap, dst_key, bank_a, bank_b, b, rs_ap=None, rs_key=None):
                op("dve", lambda e: e.tensor_tensor(out=t1[:], in0=pb[bank_a][0:64, :], in1=cs[b][:, 0, :], op=ALU.mult), reads=[PB[bank_a], "cs%d" % b], writes=["t1"])
                op("dve", lambda e: e.tensor_tensor(out=t2[:], in0=pb[bank_b][0:64, :], in1=cs[b][:, 1, :], op=ALU.mult), reads=[PB[bank_b], "cs%d" % b], writes=["t2"])
                if rs_ap is None:
                    op("dve", lambda e: e.tensor_tensor(out=dst_ap, in0=t1[:], in1=t2[:], op=ALU.add), reads=["t1", "t2"], writes=[dst_key])
                else:
                    op("dve", lambda e: e.tensor_tensor(out=t1[:], in0=t1[:], in1=t2[:], op=ALU.add), reads=["t1", "t2"], writes=["t1"])
                    op("dve", lambda e: e.tensor_tensor(out=dst_ap, in0=t1[:], in1=rs_ap, op=ALU.mult), reads=["t1", rs_key], writes=[dst_key])

            load_tile(0, xTb, 0, ropeb, 0)
            for i in range(16):
                b = i % 2
                if i + 1 < 16:
                    load_tile(i + 1, xTb, (i + 1) * 512, ropeb, (i + 1) * 512)
                tsl = slice(i * 512, (i + 1) * 512)
                tile_rstd(b, rstd[:], "rstd")
                xk = "xbt%d" % b
                mmg(1, 512, [(w1[:, k, 0:128], xbt[b][:, k, :]) for k in range(8)], [xk, "w1"])
                mmg(2, 512, [(w1[:, k, 128:192], xbt[b][:, k, :]) for k in range(8)], [xk, "w1"], m=64)
                mmg(3, 512, [(w1[:, k, 192:256], xbt[b][:, k, :]) for k in range(8)], [xk, "w1"], m=64)
                op("dve", lambda e: e.tensor_tensor(out=ckv32[:], in0=pb[1][:], in1=rstd[:], op=ALU.mult), reads=[PB[1], "rstd"], writes=["ckv32"])
                op("act", lambda e: e.activation(out=sq2[:, 0, :], in_=ckv32[:], func=AF.Square), reads=["ckv32"], writes=["sq2"])
                rms_rstd(lambda k: sq2[:, 0, :], 1, 512, r2[:], ["sq2"], "r2", 1.0 / 128, bank=4)
                op("dve", lambda e, tsl=tsl: e.scalar_tensor_tensor(out=Kn[:, tsl], in0=ckv32[:], scalar=GKV[:, 0:1], in1=r2[:], op0=ALU.mult, op1=ALU.mult),
                   reads=["ckv32", "r2", "cst"], writes=["Kn%d" % i])
                rope_to(Kr[0:64, tsl], "Kr%d" % i, 2, 3, b, rs_ap=rstd[0:64, :], rs_key="rstd")
                pbv = pb[5][:].bitcast(BF16).rearrange("p (a b) -> p a b", b=128)
                for a in range(4):
                    op("pe", lambda e, a=a, i=i: e.transpose(pbv[:, a, :], Kn[:, i * 512 + a * 128:i * 512 + (a + 1) * 128], identb[:]),
                       reads=["Kn%d" % i, "identb"], writes=[PB[5]], inc=(a == 3))
                op("act", lambda e, i=i: e.copy(out=V[:, 4 * i:4 * i + 4, :], in_=pbv[:, 0:4, :]), reads=[PB[5]], writes=["V%d" % i])

            KV_KEYS = ["Kn%d" % i for i in range(16)] + ["Kr%d" % i for i in range(16)] + ["V%d" % i for i in range(16)]
            SCALE = float(192 ** -0.5)

            for t in range(4):
                b = t % 2
                load_tile(t, xown, 1 + t * 512, ropeo, t * 512)
                osl = slice(1 + t * 512, 1 + (t + 1) * 512)
                tile_rstd(b, rstd_own[:, osl], "rstd_own")
                xk = "xbt%d" % b
                for c in range(2):
                    mmg(1 + c, 512, [(wcq[:, k, c * 128:(c + 1) * 128], xbt[b][:, k, :]) for k in range(8)], [xk, "wcq"])
                    op("dve", lambda e, c=c, osl=osl: e.tensor_tensor(out=cq32[:, c, :], in0=pb[1 + c][:], in1=rstd_own[:, osl], op=ALU.mult),
                       reads=[PB[1 + c], "rstd_own"], writes=["cq32"])
                op("act", lambda e: e.activation(out=sq2[:], in_=cq32[:], func=AF.Square), reads=["cq32"], writes=["sq2"])
                rms_rstd(lambda k: sq2[:, k, :], 2, 512, r2[:], ["sq2"], "r2", 1.0 / 256, bank=4)
                for c in range(2):
                    op("dve", lambda e, c=c: e.scalar_tensor_tensor(out=cqn[:, c, :], in0=cq32[:, c, :], scalar=GQ[:, c:c + 1], in1=r2[:], op0=ALU.mult, op1=ALU.mult),
                       reads=["cq32", "r2", "cst"], writes=["cqn"])
                for h in range(8):
                    mmg(5, 512, [(wuq[:, c, h * 192:h * 192 + 128], cqn[:, c, :]) for c in range(2)], ["wuq", "cqn"])
                    op("act", lambda e: e.copy(out=qn[:], in_=pb[5][:]), reads=[PB[5]], writes=["qn"])
                    mmg(6, 512, [(wukT[:, h, :], qn[:])], ["wukT", "qn"])
                    op("act", lambda e, h=h: e.copy(out=qabs[:, h, :], in_=pb[6][:]), reads=[PB[6]], writes=["qabs"])
                    mmg(3, 512, [(wuq[:, c, h * 192 + 128:h * 192 + 192], cqn[:, c, :]) for c in range(2)], ["wuq", "cqn"], m=64)
                    mmg(7, 512, [(wuqs[:, c, h * 64:(h + 1) * 64], cqn[:, c, :]) for c in range(2)], ["wuqs", "cqn"], m=64)
                    rope_to(qrope[0:64, h, :], "qrope", 3, 7, b)
                SPAIR = [(0, 1), (2, 3)]
                for h in range(8):
                    def s_pair(j, h=h):
                        for u in range(2):
                            kc = 2 * j + u
                            bk = SPAIR[j % 2][u]
                            ksl = slice(kc * 128, (kc + 1) * 128)
                            op("pe", lambda e, bk=bk, ksl=ksl: e.matmul(pb[bk], lhsT=Kn[:, ksl], rhs=qabs[:, h, :], start=True, stop=False),
                               reads=["Kn%d" % (kc // 4), "qabs"], writes=[PB[bk]], inc=False)
                            op("pe", lambda e, bk=bk, ksl=ksl: e.matmul(pb[bk], lhsT=Kr[:, ksl], rhs=qrope[:, h, :], start=False, stop=True),
                               reads=["Kr%d" % (kc // 4), "qrope", "Krz", "qropez"], writes=[PB[bk]], inc=(u == 1))

                    s_pair(0)
                    for j in range(32):
                        if j + 1 < 32:
                            s_pair(j + 1)
                        b0, b1 = SPAIR[j % 2]
                        P = Pp[j % 3]
                        pk = "Pp%d" % (j % 3)
                        op("act", lambda e, P=P, b0=b0: e.activation(out=P[:], in_=pbig[:, b0:b0 + 2, :], func=AF.Exp, scale=SCALE), reads=[PB[b0], PB[b1]], writes=[pk])
                        for u in range(2):
                            kc = 2 * j + u
                            op("pe", lambda e, P=P, kc=kc, u=u: e.matmul(pb[4], lhsT=V[:, kc, :], rhs=P[:, u, :], start=(kc == 0), stop=(kc == 63)),
                               reads=["V%d" % (kc // 4), pk], writes=[PB[4]], inc=False)
                        for u in range(2):
                            kc = 2 * j + u
                            op("pe", lambda e, P=P, kc=kc, u=u: e.matmul(pb[5], lhsT=onesb[:], rhs=P[:, u, :], start=(kc == 0), stop=(kc == 63)),
                               reads=["onesb", pk], writes=[PB[5]], inc=(u == 1))
                    op("dve", lambda e: e.reciprocal(out=rD[:], in_=pb[5]), reads=[PB[5]], writes=["rD"])
                    op("dve", lambda e: e.tensor_tensor(out=pc[:], in0=pb[4], in1=rD[:], op=ALU.mult), reads=[PB[4], "rD"], writes=["pc"])
                    mmg(6, 512, [(wuv[:, h, :], pc[:])], ["wuv", "pc"])
                    op("act", lambda e, h=h, t=t: e.copy(out=attn[:, h, t * 512:(t + 1) * 512], in_=pb[6]), reads=[PB[6]], writes=["attn"])
            sch.barrier()
            sch.emit()

        PIECES = [(0, 512), (512, 512), (1024, 512), (1536, 512), (2048, 2)]
        with ExitStack() as p2:
            xbo = sbt(p2, "xbo", [128, 8, TOK + 2], BF16)
            ycin = sbt(p2, "ycin", [128, 8, TOK], BF16)
            ta = sbt(p2, "ta", [128, 512], F32)
            tb = sbt(p2, "tb", [128, 512], F32)
            hsq = sbt(p2, "hsq", [128, 8, 2], BF16)
            p2c = ExitStack()
            wc = [sbt(p2c, "wc%d" % i, [128, 8, 384], BF16) for i in range(2)]
            v = sbt(p2c, "v", [128, TOK + 2], F32)
            gbv = sbt(p2c, "gbv", [128, TOK + 2], F32)
            c1 = sbt(p2c, "c1", [128, TOK], F32)
            c2 = sbt(p2c, "c2", [128, TOK], F32)

            dma("pool", lambda e: e.dma_start(out=xbo[:], in_=xown.rearrange("(k p) t -> p k t", p=128)), "xbo", writes=["xbo"])
            for hc, col in enumerate((0, TOK + 1)):
                op("act", lambda e, hc=hc, col=col: e.activation(out=hsq[:, :, hc:hc + 1], in_=xbo[:, :, col:col + 1], func=AF.Square), reads=["xbo"], writes=["hsq"])
            for hc, col in enumerate((0, TOK + 1)):
                rms_rstd(lambda k, hc=hc: hsq[:, k, hc:hc + 1], 8, 1, rstd_own[:, col:col + 1], ["hsq"], "rstd_own", 1.0 / D, bank=0)

            def load_wc(j):
                t_ = wc[j % 2]
                key = "wc%d" % (j % 2)
                for g_ in range(3):
                    dma("pool", lambda e, g_=g_: e.dma_start(out=t_[:, :, g_ * 128:(g_ + 1) * 128],
                                                          in_=w_in[:, g_ * 1024 + j * 128:g_ * 1024 + (j + 1) * 128].rearrange("(k p) c -> p k c", p=128)),
                        "w_" + key, writes=[key])
                op("pool", lambda e: e.tensor_tensor(out=t_[:], in0=t_[:], in1=bc(GMIX, 384), op=ALU.mult), reads=[key, "cst"], writes=[key])

            load_wc(0)
            for j in range(8):
                if j + 1 < 8:
                    load_wc(j + 1)
                W = wc[j % 2]
                wk = "wc%d" % (j % 2)
                for (c0, n) in PIECES:
                    psl = slice(c0, c0 + n)
                    mmg(1, n, [(W[:, k, 0:128], xbo[:, k, psl]) for k in range(8)], [wk, "xbo"])
                    mmg(2, n, [(W[:, k, 128:256], xbo[:, k, psl]) for k in range(8)], [wk, "xbo"])
                    mmg(3, n, [(W[:, k, 256:384], xbo[:, k, psl]) for k in range(8)], [wk, "xbo"])
                    op("dve", lambda e, n=n, psl=psl: e.tensor_tensor(out=ta[:, 0:n], in0=pb[1][:, 0:n], in1=rstd_own[:, psl], op=ALU.mult), reads=[PB[1], "rstd_own"], writes=["ta"])
                    op("dve", lambda e, n=n, psl=psl: e.tensor_tensor(out=tb[:, 0:n], in0=pb[3][:, 0:n], in1=rstd_own[:, psl], op=ALU.mult), reads=[PB[3], "rstd_own"], writes=["tb"])
                    op("dve", lambda e, n=n, psl=psl: e.tensor_tensor(out=v[:, psl], in0=ta[:, 0:n], in1=tb[:, 0:n], op=ALU.mult), reads=["ta", "tb"], writes=["v"])
                    op("dve", lambda e, n=n, psl=psl: e.tensor_tensor(out=gbv[:, psl], in0=pb[2][:, 0:n], in1=rstd_own[:, psl], op=ALU.mult), reads=[PB[2], "rstd_own"], writes=["gbv"])
                op("pool", lambda e, j=j: e.tensor_scalar(out=c1[:], in0=v[:, 0:TOK], scalar1=CW[0][:, j:j + 1], scalar2=CB[:, j:j + 1], op0=ALU.mult, op1=ALU.add),
                   reads=["v", "cst"], writes=["c1"])
                op("dve", lambda e, j=j: e.scalar_tensor_tensor(out=c2[:], in0=v[:, 1:TOK + 1], scalar=CW[1][:, j:j + 1], in1=c1[:], op0=ALU.mult, op1=ALU.add),
                   reads=["v", "c1", "cst"], writes=["c2"])
                op("dve", lambda e, j=j: e.scalar_tensor_tensor(out=c1[:], in0=v[:, 2:TOK + 2], scalar=CW[2][:, j:j + 1], in1=c2[:], op0=ALU.mult, op1=ALU.add),
                   reads=["v", "c2", "cst"], writes=["c1"])
                op("pool", lambda e, j=j: e.tensor_tensor(out=ycin[:, j, :], in0=c1[:], in1=gbv[:, 1:TOK + 1], op=ALU.mult), reads=["c1", "gbv"], writes=["ycin"])

            sch.barrier()
            sch.emit()
            p2c.close()
            wm = [sbt(p2, "wm%d" % i, [128, 8, 512], BF16) for i in range(2)]
            sga = sbt(p2, "sga", [128, 512], F32)
            sgb = sbt(p2, "sgb", [128, 512], F32)

            def load_wm(j):
                t_ = wm[j % 2]
                key = "wm%d" % (j % 2)
                jsl = slice(j * 128, (j + 1) * 128)
                dma("pool", lambda e: e.dma_start(out=t_[:, :, 0:128], in_=w_conv_out[:, jsl].rearrange("(k p) c -> p k c", p=128)), "w_" + key, writes=[key])
                dma("pool", lambda e: e.dma_start(out=t_[:, :, 128:256], in_=w_mla_out[:, jsl].rearrange("(k p) c -> p k c", p=128)), "w_" + key, writes=[key])
                dma("pool", lambda e: e.dma_start(out=t_[:, :, 256:384], in_=w_in[:, 3520 + j * 128:3520 + (j + 1) * 128].rearrange("(k p) c -> p k c", p=128)), "w_" + key, writes=[key])
                dma("pool", lambda e: e.dma_start(out=t_[:, :, 384:512], in_=w_in[:, 4544 + j * 128:4544 + (j + 1) * 128].rearrange("(k p) c -> p k c", p=128)), "w_" + key, writes=[key])
                op("pool", lambda e: e.tensor_tensor(out=t_[:, :, 256:512], in0=t_[:, :, 256:512], in1=bc(GMIX, 256), op=ALU.mult), reads=[key, "cst"], writes=[key])

            load_wm(0)
            for j in range(8):
                if j + 1 < 8:
                    load_wm(j + 1)
                W = wm[j % 2]
                wk = "wm%d" % (j % 2)
                for t in range(4):
                    tsl = slice(t * 512, (t + 1) * 512)
                    osl = slice(1 + t * 512, 1 + (t + 1) * 512)
                    mmg(1, 512, [(W[:, k, 0:128], ycin[:, k, tsl]) for k in range(8)], [wk, "ycin"])
                    mmg(2, 512, [(W[:, k, 128:256], attn[:, k, tsl]) for k in range(8)], [wk, "attn"])
                    mmg(3, 512, [(W[:, k, 256:384], xbo[:, k, osl]) for k in range(8)], [wk, "xbo"])
                    mmg(4, 512, [(W[:, k, 384:512], xbo[:, k, osl]) for k in range(8)], [wk, "xbo"])
                    op("dve", lambda e, osl=osl: e.tensor_tensor(out=ta[:], in0=pb[3][:], in1=rstd_own[:, osl], op=ALU.mult), reads=[PB[3], "rstd_own"], writes=["ta"])
                    op("act", lambda e, j=j: e.activation(out=sga[:], in_=ta[:], func=AF.Sigmoid, bias=BG[:, j:j + 1]), reads=["ta", "cst"], writes=["sga"])
                    op("dve", lambda e, osl=osl: e.tensor_tensor(out=tb[:], in0=pb[4][:], in1=rstd_own[:, osl], op=ALU.mult), reads=[PB[4], "rstd_own"], writes=["tb"])
                    op("act", lambda e, j=j: e.activation(out=sgb[:], in_=tb[:], func=AF.Sigmoid, bias=BG[:, 8 + j:9 + j]), reads=["tb", "cst"], writes=["sgb"])
                    op("dve", lambda e: e.tensor_tensor(out=sga[:], in0=pb[1][:], in1=sga[:], op=ALU.mult), reads=[PB[1], "sga"], writes=["sga"])
                    op("dve", lambda e: e.tensor_tensor(out=sgb[:], in0=pb[2][:], in1=sgb[:], op=ALU.mult), reads=[PB[2], "sgb"], writes=["sgb"])
                    op("dve", lambda e, j=j, tsl=tsl: e.tensor_tensor(out=mixed[:, j, tsl], in0=sga[:], in1=sgb[:], op=ALU.add), reads=["sga", "sgb"], writes=["mixed"])
            sch.barrier()
            sch.emit()

        with ExitStack() as p2b:
            wx = [sbt(p2b, "wx%d" % i, [128, 8, 128], BF16) for i in range(2)]
            dma("sp", lambda e: e.dma_start(out=x1[:], in_=xown[:, 1:TOK + 1].rearrange("(k p) t -> p k t", p=128)), "x1ld", writes=["x1"])

            def load_wx(j):
                load_w(wx[j % 2], w_mix_out[:, j * 128:(j + 1) * 128], "wx%d" % (j % 2))
            load_wx(0)
            for j in range(8):
                if j + 1 < 8:
                    load_wx(j + 1)
                for t in range(4):
                    tsl = slice(t * 512, (t + 1) * 512)
                    bk = 1 + (t % 2)
                    mmg(bk, 512, [(wx[j % 2][:, k, :], mixed[:, k, tsl]) for k in range(8)], ["wx%d" % (j % 2), "mixed"])
                    op("dve", lambda e, j=j, tsl=tsl, bk=bk: e.tensor_tensor(out=x1[:, j, tsl], in0=pb[bk][:], in1=x1[:, j, tsl], op=ALU.add), reads=[PB[bk], "x1"], writes=["x1"])
            if debug:
                dma("sp", lambda e: e.dma_start(out=dbg["x1"].rearrange("(k p) t -> p k t", p=128), in_=x1[:]), "dbg", reads=["x1"])
            op("pool", lambda e: e.memset(zt[:], 0.0), writes=["zt"])
            for z in range(32):
                dma("sp", lambda e, z=z: e.dma_start(out=Dd[z * 256:(z + 1) * 256, :].rearrange("(p a) d -> p (a d)", a=2), in_=zt[:]), "zD", reads=["zt"])
            sch.barrier(skip=("zD",))
            sch.emit()

        with ExitStack() as p3:
            K2T = sbt(p3, "K2T", [128, 8, 256], BF16)
            V2 = sbt(p3, "V2", [128, 2, D], BF16)
            hsq3 = sbt(p3, "hsq3", [128, 8, 512], BF16)
            rs3 = sbt(p3, "rs3", [128, 512], F32)
            SC2 = float(256 ** -0.5)
            w2q = sbt(p3, "w2q", [128, 8, D], BF16)
            w2o = sbt(p3, "w2o", [128, 8, D], BF16)
            with ExitStack() as p3a:
                wkv = sbt(p3a, "wkv", [128, 8, 2048], BF16)
                mem32 = sbt(p3a, "mem32", [128, 8, 256], F32)
                memn = sbt(p3a, "memn", [128, 8, 256], BF16)
                load_w(wkv, w_mem_kv, "wkv")
                load_w(w2q, w_mem_q, "w2q")
                load_w(w2o, w_mem_out, "w2o")
                dma("sp", lambda e: e.dma_start(out=mem32[:], in_=memT.rearrange("(k p) t -> p k t", p=128)), "mem", writes=["mem32"])
                op("act", lambda e: e.activation(out=hsq3[:, :, 0:256], in_=mem32[:], func=AF.Square), reads=["mem32"], writes=["hsq3"])
                rms_rstd(lambda k: hsq3[:, k, 0:256], 8, 256, rs3[:, 0:256], ["hsq3"], "rs3", 1.0 / D, bank=0)
                for k in range(8):
                    op("dve", lambda e, k=k: e.scalar_tensor_tensor(out=memn[:, k, :], in0=mem32[:, k, :], scalar=GMKV[:, k:k + 1], in1=rs3[:, 0:256], op0=ALU.mult, op1=ALU.mult),
                       reads=["mem32", "rs3", "cst"], writes=["memn"])
                for m in range(8):
                    bk = 1 + (m % 2)
                    mmg(bk, 256, [(wkv[:, k, m * 128:(m + 1) * 128], memn[:, k, :]) for k in range(8)], ["wkv", "memn"])
                    op("act", lambda e, m=m, bk=bk: e.copy(out=K2T[:, m, :], in_=pb[bk][:, 0:256]), reads=[PB[bk]], writes=["K2T"])
                for kc in range(2):
                    for nh in range(2):
                        bk = 3 + nh
                        mmg(bk, 512, [(memn[:, k, kc * 128:(kc + 1) * 128], wkv[:, k, 1024 + nh * 512:1024 + (nh + 1) * 512]) for k in range(8)], ["wkv", "memn"])
                        op("act", lambda e, kc=kc, nh=nh, bk=bk: e.copy(out=V2[:, kc, nh * 512:(nh + 1) * 512], in_=pb[bk][:]), reads=[PB[bk]], writes=["V2"])
                sch.barrier(skip=("zD",))
                sch.emit()
            h2 = sbt(p3, "h2", [128, 8, 512], BF16)
            q2 = sbt(p3, "q2", [128, 8, 512], BF16)
            o2 = sbt(p3, "o2", [128, 8, 512], BF16)
            P2 = [sbt(p3, "P2_%d" % i, [128, 512], BF16) for i in range(2)]
            rD2 = sbt(p3, "rD2", [128, 512], F32)

            for t in range(4):
                tsl = slice(t * 512, (t + 1) * 512)
                op("act", lambda e, tsl=tsl: e.activation(out=hsq3[:], in_=x1[:, :, tsl], func=AF.Square), reads=["x1"], writes=["hsq3"])
                rms_rstd(lambda k: hsq3[:, k, :], 8, 512, rs3[:], ["hsq3"], "rs3", 1.0 / D, bank=0)
                for k in range(8):
                    op("dve", lambda e, k=k, tsl=tsl: e.scalar_tensor_tensor(out=h2[:, k, :], in0=x1[:, k, tsl], scalar=GMEM[:, k:k + 1], in1=rs3[:], op0=ALU.mult, op1=ALU.mult),
                       reads=["x1", "rs3", "cst"], writes=["h2"])
                for m in range(8):
                    bk = 1 + (m % 2)
                    mmg(bk, 512, [(w2q[:, k, m * 128:(m + 1) * 128], h2[:, k, :]) for k in range(8)], ["w2q", "h2"])
                    op("act", lambda e, m=m, bk=bk: e.copy(out=q2[:, m, :], in_=pb[bk][:]), reads=[PB[bk]], writes=["q2"])
                for h in range(4):
                    for kc in range(2):
                        bk = 3 + kc
                        mmg(bk, 512, [(K2T[:, 2 * h + c, kc * 128:(kc + 1) * 128], q2[:, 2 * h + c, :]) for c in range(2)], ["K2T", "q2"])
                        op("act", lambda e, kc=kc, bk=bk: e.activation(out=P2[kc][:], in_=pb[bk][:], func=AF.Exp, scale=SC2), reads=[PB[bk]], writes=["P2_%d" % kc])
                    mmg(5, 512, [(onesb[:], P2[kc][:]) for kc in range(2)], ["onesb", "P2_0", "P2_1"])
                    op("dve", lambda e: e.reciprocal(out=rD2[:], in_=pb[5][:]), reads=[PB[5]], writes=["rD2"])
                    for dc in range(2):
                        bk = 6 + dc
                        mmg(bk, 512, [(V2[:, kc, h * 256 + dc * 128:h * 256 + (dc + 1) * 128], P2[kc][:]) for kc in range(2)], ["V2", "P2_0", "P2_1"])
                        op("dve", lambda e, h=h, dc=dc, bk=bk: e.tensor_tensor(out=o2[:, 2 * h + dc, :], in0=pb[bk][:], in1=rD2[:], op=ALU.mult), reads=[PB[bk], "rD2"], writes=["o2"])
                for m in range(8):
                    bk = 1 + (m % 2)
                    mmg(bk, 512, [(w2o[:, k, m * 128:(m + 1) * 128], o2[:, k, :]) for k in range(8)], ["w2o", "o2"])
                    op("dve", lambda e, m=m, tsl=tsl, bk=bk: e.tensor_tensor(out=x1[:, m, tsl], in0=pb[bk][:], in1=x1[:, m, tsl], op=ALU.add), reads=[PB[bk], "x1"], writes=["x1"])
            sch.barrier(skip=("zD",))
            sch.emit()

        pr = ExitStack()
        trib = sbt(pr, "trib", [128, 128], BF16)
        iot = sbt(pr, "iot", [128, 16], F32)
        iots = sbt(pr, "iots", [128, 1024], F32)
        myrows = sbt(pr, "myrows", [128, 4], I32)
        gsel = sbt(pr, "gsel", [128, 4, 16], F32)
        aff4 = sbt(pr, "aff4", [128, 4, 64], F32)
        lo = sbt(pr, "lo", [128, 4], F32)
        mid = sbt(pr, "mid", [128, 4], F32)
        cmp = sbt(pr, "cmp", [128, 4, 64], F32)
        csa = sbt(pr, "csa", [128, 4, 64], F32)
        cntb = sbt(pr, "cntb", [128, 4], BF16)
        cnt32 = sbt(pr, "cnt32", [128, 4], F32)
        ge = sbt(pr, "ge", [128, 4], F32)
        off32 = sbt(pr, "off32", [128, 4], F32)
        offa = sbt(pr, "offa", [128, 4], BF16)
        offa32 = sbt(pr, "offa32", [128, 4], F32)
        endp = sbt(pr, "endp", [128, 4], F32)
        rowsR = sbt(pr, "rowsR", [128, 4, 68], BF16)
        ohA = sbt(pr, "ohA", [128, 1024], F32)
        OH = sbt(pr, "OH", [128, 1024], BF16)
        offs = sbt(pr, "offs", [128, 8], F32)
        meta = sbt(pr, "meta", [128, 4, 4], F32)
        idxh = sbt(pr, "idxh", [128, 4, 8], I32)
        idxhf = sbt(pr, "idxhf", [128, 8], F32)
        jl = sbt(pr, "jl", [128, 8], F32)
        le = sbt(pr, "le", [128, 8, 64], F32)
        fi = sbt(pr, "fi", [128, 8], F32)
        idxf = sbt(pr, "idxf", [128, 8], F32)
        idx = sbt(pr, "idx", [128, 4, 8], I32)
        wg0 = sbt(pr, "wg0", [128, 8, D], BF16)
        wu0 = sbt(pr, "wu0", [128, 8, D], BF16)
        with ExitStack() as p4:
            wr = sbt(p4, "wr", [128, 8, 16], BF16)
            hsq4_ = [sbt(p4, "hsq4_%d" % i, [128, 8, 512], BF16) for i in range(2)]
            rs4_ = [sbt(p4, "rs4_%d" % i, [128, 512], F32) for i in range(2)]
            h3_ = [sbt(p4, "h3_%d" % i, [128, 8, 512], BF16) for i in range(2)]
            rows = [sbt(p4, "rows%d" % i, [128, 1024], BF16) for i in range(2)]
            aftm = [sbt(p4, "aftm%d" % i, [128, 4, 16], F32) for i in range(2)]
            mx4 = sbt(p4, "mx4", [128, 4], F32)
            sm4 = sbt(p4, "sm4", [128, 4], F32)
            sh4 = sbt(p4, "sh4", [128, 4, 16], F32)
            x2tm = [sbt(p4, "x2tm%d" % i, [128, D], F32) for i in range(2)]
            affT = sbt(p4, "affT", [16, TOK], F32)
            eT = sbt(p4, "eT", [16, 512], F32)
            rsT = sbt(p4, "rsT", [16, 512], F32)
            ones16 = sbt(p4, "ones16", [16, 16], F32)
            load_w(wr, w_router, "wr")
            dma("pool", lambda e: e.dma_start(out=trib[:], in_=tri_d), "c3", writes=["trib"])
            dma("sp", lambda e: e.dma_start(out=iot[:], in_=iot_d), "c4", writes=["iot"])
            dma("sp", lambda e: e.dma_start(out=iots[:], in_=iots_d), "c5", writes=["iots"])
            dma("sp", lambda e: e.dma_start(out=myrows[:], in_=myrows_d), "c6", writes=["myrows"])
            dma("sp", lambda e: e.dma_start(out=gsel[:], in_=gsel_d), "c7", writes=["gsel"])
            op("dve", lambda e: e.memset(ones16[:], 1.0), writes=["ones16"])
            ci = 0
            deferred_ag = []

            def hall_ag(a_):
                dma("pool", lambda e: e.collective_compute("AllGather", ALU.bypass, replica_groups=GROUPS, ins=[Hloc[a_ * 256:(a_ + 1) * 256, :]],
                                                           outs=[Hall[a_ * 1024:(a_ + 1) * 1024, :]]), "cc", reads=["Hloc_s%d" % (2 * a_), "Hloc_s%d" % (2 * a_ + 1)], writes=["Hall%d" % a_], inc=1)

            def start_routing_inputs():
                dma("sp", lambda e: e.dma_start(out=Aloc, in_=affT[:]), "st_aff", reads=["affT"], writes=["Aloc"])
                if os.environ.get("KSKIP_CC") == "1":
                    dma("pool", lambda e: e.dma_start(out=Aall[0:16, :], in_=Aloc), "cc", reads=["Aloc"], writes=["Aall"])
                else:
                    dma("pool", lambda e: e.collective_compute("AllGather", ALU.bypass, replica_groups=GROUPS, ins=[Aloc], outs=[Aall]), "cc2", reads=["Aloc"], writes=["Aall"], inc=1)
                Aall2 = Aall.rearrange("r (i f) -> (r i) f", f=64)
                for k in range(4):
                    dma("pool", lambda e, k=k: e.indirect_dma_start(out=aff4[:, k, :], out_offset=None, in_=Aall2,
                                                                   in_offset=bass.IndirectOffsetOnAxis(ap=myrows[:, k:k + 1], axis=0)),
                        "ga", reads=["Aall", "myrows"], writes=["aff4"])

            def norm3(t):
                tsl = slice(t * 512, (t + 1) * 512)
                hq, rs_, h_ = hsq4_[t % 2], rs4_[t % 2], h3_[t % 2]
                kq, kr_, kh = "hsq4_%d" % (t % 2), "rs4_%d" % (t % 2), "h3_%d" % (t % 2)
                op("act", lambda e: e.activation(out=hq[:], in_=x1[:, :, tsl], func=AF.Square), reads=["x1"], writes=[kq])
                rms_rstd(lambda k: hq[:, k, :], 8, 512, rs_[:], [kq], kr_, 1.0 / D, bank=0)
                for k in range(8):
                    op("dve", lambda e, k=k: e.scalar_tensor_tensor(out=h_[:, k, :], in0=x1[:, k, tsl], scalar=GMOE[:, k:k + 1], in1=rs_[:], op0=ALU.mult, op1=ALU.mult),
                       reads=["x1", kr_, "cst"], writes=[kh])
                mmg(1, 512, [(wr[:, k, :], h_[:, k, :]) for k in range(8)], ["wr", kh], m=16)
                op("act", lambda e: e.activation(out=eT[:], in_=pb[1][0:16, :], func=AF.Exp), reads=[PB[1]], writes=["eT"])
                op("pe", lambda e: e.matmul(pb[2][0:16, :], lhsT=ones16[:], rhs=eT[:], start=True, stop=True), reads=["ones16", "eT"], writes=[PB[2]])
                op("dve", lambda e: e.reciprocal(out=rsT[:], in_=pb[2][0:16, :]), reads=[PB[2]], writes=["rsT"])
                op("dve", lambda e: e.tensor_tensor(out=affT[:, tsl], in0=eT[:], in1=rsT[:], op=ALU.mult), reads=["eT", "rsT"], writes=["affT"])
                if t == 3:
                    start_routing_inputs()
            norm3(0)
            for t in range(4):
                tsl = slice(t * 512, (t + 1) * 512)
                if t + 1 < 4:
                    norm3(t + 1)
                h3 = h3_[t % 2]
                H3K = "h3_%d" % (t % 2)
                for sc in range(4):
                    csl = slice(sc * 128, (sc + 1) * 128)
                    mmg(3, 16, [(h3[:, k, csl], wr[:, k, :]) for k in range(8)], ["wr", H3K], col0=sc * 16)
                lg = pb[3][:, 0:64].rearrange("p (s e) -> p s e", e=16)
                AT = aftm[t % 2]
                ak = "aftm%d" % (t % 2)
                op("dve", lambda e: e.reduce_max(out=mx4[:], in_=lg, axis=AX.X), reads=[PB[3]], writes=["mx4"])
                op("dve", lambda e: e.tensor_tensor(out=sh4[:], in0=lg, in1=bc(mx4[:], 16), op=ALU.subtract), reads=[PB[3], "mx4"], writes=["sh4"])
                op("act", lambda e: e.activation(out=sh4[:], in_=sh4[:], func=AF.Exp), reads=["sh4"], writes=["sh4"])
                op("dve", lambda e: e.reduce_sum(out=sm4[:], in_=sh4[:], axis=AX.X), reads=["sh4"], writes=["sm4"])
                op("dve", lambda e: e.reciprocal(out=sm4[:], in_=sm4[:]), reads=["sm4"], writes=["sm4"])
                op("dve", lambda e, AT=AT: e.tensor_tensor(out=AT[:], in0=sh4[:], in1=bc(sm4[:], 16), op=ALU.mult), reads=["sh4", "sm4"], writes=[ak])
                dma("sp", lambda e, AT=AT, t=t: e.dma_start(out=Gloc[t * 512:(t + 1) * 512, :].rearrange("(s p) e -> p s e", p=128), in_=AT[:]), "st_" + ak, reads=[ak], writes=["Gloc_" + ak])
                for sc in range(4):
                    rb = ci % 2
                    ci += 1
                    R_ = rows[rb]
                    rk = "rows%d" % rb
                    X_ = x2tm[rb]
                    xk = "x2tm%d" % rb
                    csl = slice(sc * 128, (sc + 1) * 128)
                    gsl = slice(t * 512 + sc * 128, t * 512 + (sc + 1) * 128)
                    brow = 4 if rb == 0 else 7
                    bx = (5, 6) if rb == 0 else (1, 2)
                    pbt = pb[brow].bitcast(BF16).rearrange("p (a b) -> p a b", b=128)
                    for k in range(8):
                        op("pe", lambda e, k=k, csl=csl, pbt=pbt, h3=h3: e.transpose(pbt[:, k, :], h3[:, k, csl], identb[:]), reads=[H3K, "identb"], writes=[PB[brow]], inc=(k == 7))
                    op("act", lambda e, R_=R_, brow=brow: e.copy(out=R_[:], in_=pb[brow].bitcast(BF16)), reads=[PB[brow]], writes=[rk])
                    dma("sp", lambda e, R_=R_, gsl=gsl: e.dma_start(out=Hloc[gsl, :], in_=R_[:]), "st_" + rk, reads=[rk], writes=["Hloc_s%d" % (ci - 1)])
                    if ci % 2 == 0 and os.environ.get("KSKIP_CC") != "1":
                        a_ = ci // 2 - 1
                        if a_ <= 7:
                            hall_ag(a_)
                        else:
                            deferred_ag.append(a_)
                    for k in range(8):
                        bk = bx[k // 4]
                        op("pe", lambda e, k=k, bk=bk, gsl=gsl: e.transpose(pb[bk][:, (k % 4) * 128:(k % 4 + 1) * 128], x1[:, k, gsl], identf[:]), reads=["x1", "identf"], writes=[PB[bk]], inc=(k % 4 == 3))
                    op("dve", lambda e, X_=X_, bx=bx: e.tensor_copy(out=X_[:, 0:512], in_=pb[bx[0]]), reads=[PB[bx[0]]], writes=[xk])
                    op("dve", lambda e, X_=X_, bx=bx: e.tensor_copy(out=X_[:, 512:1024], in_=pb[bx[1]]), reads=[PB[bx[1]]], writes=[xk])
                    dma("sp", lambda e, X_=X_, gsl=gsl: e.dma_start(out=X2loc[gsl, :], in_=X_[:]), "st_" + xk, reads=[xk], writes=["X2loc_" + xk])
            if os.environ.get("KSKIP_CC") != "1":
                for a_ in deferred_ag:
                    hall_ag(a_)
                dma("pool", lambda e: e.collective_compute("AllGather", ALU.bypass, replica_groups=GROUPS, ins=[Gloc], outs=[Gall]), "cc4", reads=["Gloc_aftm0", "Gloc_aftm1"], writes=["Gall"], inc=1)
            if debug:
                dma("sp", lambda e: e.dma_start(out=dbg["x2"], in_=X2loc), "dbg", reads=["X2loc_x2tm0", "X2loc_x2tm1"])
            load_w(wg0, w_eg[0], "wg0")
            load_w(wu0, w_eu[0], "wu0")
            op("dve", lambda e: e.memset(lo[:], 0.0), writes=["lo"])
            for it in range(32):
                step = float(2.0 ** -(it + 1))
                op("dve", lambda e, step=step: e.tensor_scalar(out=mid[:], in0=lo[:], scalar1=step, scalar2=None, op0=ALU.add), reads=["lo"], writes=["mid"])
                op("dve", lambda e: e.tensor_tensor(out=cmp[:], in0=aff4[:], in1=bc(mid[:], 64), op=ALU.is_ge), reads=["aff4", "mid"], writes=["cmp"])
                op("dve", lambda e: e.reduce_sum(out=cnt32[:], in_=cmp[:], axis=AX.X), reads=["cmp"], writes=["cnt32"])
                op("dve", lambda e: e.tensor_copy(out=cntb[:], in_=cnt32[:]), reads=["cnt32"], writes=["cntb"])
                op("pe", lambda e: e.matmul(pb[0][:, 0:4], lhsT=onesb[:], rhs=cntb[:], start=True, stop=True), reads=["onesb", "cntb"], writes=[PB[0]])
                op("dve", lambda e, step=step: e.tensor_scalar(out=ge[:], in0=pb[0][:, 0:4], scalar1=1023.5, scalar2=step, op0=ALU.is_ge, op1=ALU.mult), reads=[PB[0]], writes=["ge"])
                op("dve", lambda e: e.tensor_tensor(out=lo[:], in0=lo[:], in1=ge[:], op=ALU.add), reads=["lo", "ge"], writes=["lo"])
            op("dve", lambda e: e.tensor_tensor(out=cmp[:], in0=aff4[:], in1=bc(lo[:], 64), op=ALU.is_ge), reads=["aff4", "lo"], writes=["cmp"])
            op("dve", lambda e: e.reduce_sum(out=cnt32[:], in_=cmp[:], axis=AX.X), reads=["cmp"], writes=["cnt32"])
            op("dve", lambda e: e.tensor_copy(out=cntb[:], in_=cnt32[:]), reads=["cnt32"], writes=["cntb"])
            op("pe", lambda e: e.matmul(pb[0][:, 0:4], lhsT=trib[:], rhs=cntb[:], start=True, stop=True), reads=["trib", "cntb"], writes=[PB[0]])
            op("dve", lambda e: e.tensor_copy(out=off32[:], in_=pb[0][:, 0:4]), reads=[PB[0]], writes=["off32"])
            op("dve", lambda e: e.tensor_copy(out=offa[:], in_=off32[:]), reads=["off32"], writes=["offa"])
            op("dve", lambda e: e.tensor_copy(out=offa32[:], in_=offa[:]), reads=["offa"], writes=["offa32"])
            op("dve", lambda e: e.tensor_tensor(out=endp[:], in0=off32[:], in1=cnt32[:], op=ALU.add), reads=["off32", "cnt32"], writes=["endp"])
            src, dst, srck, dstk = cmp, csa, "cmp", "csa"
            for s_ in (1, 2, 4, 8, 16, 32):
                op("dve", lambda e, s_=s_, src=src, dst=dst: e.tensor_copy(out=dst[:, :, 0:s_], in_=src[:, :, 0:s_]), reads=[srck], writes=[dstk])
                op("dve", lambda e, s_=s_, src=src, dst=dst: e.tensor_tensor(out=dst[:, :, s_:64], in0=src[:, :, s_:64], in1=src[:, :, 0:64 - s_], op=ALU.add), reads=[srck], writes=[dstk])
                src, dst, srck, dstk = dst, src, dstk, srck
            csf, csk = src, srck
            op("dve", lambda e: e.tensor_copy(out=rowsR[:, :, 0:64], in_=csf[:]), reads=[csk], writes=["rowsR"])
            op("dve", lambda e: e.tensor_copy(out=rowsR[:, :, 64:65], in_=offa[:].unsqueeze(2)), reads=["offa"], writes=["rowsR"])
            op("dve", lambda e: e.tensor_tensor(out=rowsR[:, :, 65:66], in0=off32[:].unsqueeze(2), in1=offa32[:].unsqueeze(2), op=ALU.subtract), reads=["off32", "offa32"], writes=["rowsR"])
            op("dve", lambda e: e.tensor_copy(out=rowsR[:, :, 66:67], in_=iot[:, 8:9].unsqueeze(1).to_broadcast([128, 4, 1])), reads=["iot"], writes=["rowsR"])
            op("dve", lambda e: e.tensor_copy(out=rowsR[:, :, 67:68], in_=iot[:, 9:10].unsqueeze(1).to_broadcast([128, 4, 1])), reads=["iot"], writes=["rowsR"])
            if debug:
                dma("sp", lambda e: e.dma_start(out=dbg["thr"], in_=lo[:]), "dbg", reads=["lo"])
            pR = [pb[1], pb[2]]
            for k in range(4):
                op("dve", lambda e, k=k: e.tensor_scalar(out=ohA[:], in0=iots[:], scalar1=off32[:, k:k + 1], scalar2=None, op0=ALU.is_ge), reads=["iots", "off32"], writes=["ohA"])
                op("dve", lambda e, k=k: e.scalar_tensor_tensor(out=OH[:], in0=iots[:], scalar=endp[:, k:k + 1], in1=ohA[:], op0=ALU.is_lt, op1=ALU.mult), reads=["iots", "endp", "ohA"], writes=["OH"])
                for c in range(8):
                    bk = 1 + c // 4
                    op("pe", lambda e, c=c, k=k, bk=bk: e.matmul(pb[bk][:, (c % 4) * 128:(c % 4) * 128 + 68], lhsT=OH[:, c * 128:(c + 1) * 128], rhs=rowsR[:, k, :], start=True, stop=True),
                       reads=["OH", "rowsR"], writes=[PB[bk]])
                for hb in range(2):
                    pv = pb[1 + hb][:].rearrange("p (c w) -> p c w", w=128)
                    hs = slice(hb * 4, hb * 4 + 4)
                    op("dve", lambda e, pv=pv: e.tensor_copy(out=meta[:], in_=pv[:, :, 64:68]), reads=[PB[1 + hb]], writes=["meta"])
                    op("dve", lambda e, hs=hs: e.tensor_tensor(out=offs[:, hs].unsqueeze(2), in0=meta[:, :, 0:1], in1=meta[:, :, 1:2], op=ALU.add), reads=["meta"], writes=["offs"])
                    op("dve", lambda e, hs=hs: e.tensor_tensor(out=jl[:, hs], in0=iot[:, hs], in1=offs[:, hs], op=ALU.subtract), reads=["iot", "offs"], writes=["jl"])
                    op("dve", lambda e, pv=pv, hs=hs: e.tensor_tensor(out=le[:, hs, :], in0=pv[:, :, 0:64], in1=bc(jl[:, hs], 64), op=ALU.is_le), reads=[PB[1 + hb], "jl"], writes=["le"])
                    op("dve", lambda e, hs=hs: e.reduce_sum(out=fi[:, hs], in_=le[:, hs, :], axis=AX.X), reads=["le"], writes=["fi"])
                    op("dve", lambda e, hs=hs: e.scalar_tensor_tensor(out=idxf[:, hs].unsqueeze(2), in0=meta[:, :, 2:3], scalar=64.0, in1=fi[:, hs].unsqueeze(2), op0=ALU.mult, op1=ALU.add),
                       reads=["meta", "fi"], writes=["idxf"])
                    op("dve", lambda e, hs=hs: e.scalar_tensor_tensor(out=idxhf[:, hs].unsqueeze(2), in0=meta[:, :, 3:4], scalar=64.0, in1=fi[:, hs].unsqueeze(2), op0=ALU.mult, op1=ALU.add),
                       reads=["meta", "fi"], writes=["idxhf"])
                op("dve", lambda e, k=k: e.tensor_copy(out=idx[:, k, :], in_=idxf[:]), reads=["idxf"], writes=["idx"])
                op("dve", lambda e, k=k: e.tensor_copy(out=idxh[:, k, :], in_=idxhf[:]), reads=["idxhf"], writes=["idxh"])
            if debug:
                dma("sp", lambda e: e.dma_start(out=dbg["idx"], in_=idx[:].rearrange("p a b -> p (a b)")), "dbg", reads=["idx"])
            sch.barrier()
            sch.emit()

        with ExitStack() as p5:
            xrow = [sbt(p5, "xrow%d" % i, [128, 1024], BF16) for i in range(8)]
            gate = sbt(p5, "gate", [128, 4, 8], F32)
            gt4 = sbt(p5, "gt4", [128, 4, 8, 16], F32)
            gtm = sbt(p5, "gtm", [128, 8, 16], F32)
            xgT = sbt(p5, "xgT", [128, 8, 1024], BF16)
            aT = sbt(p5, "aT", [128, 8, 1024], BF16)
            wg = [wg0, sbt(p5, "wg1", [128, 8, D], BF16)]
            wu = [wu0, sbt(p5, "wu1", [128, 8, D], BF16)]
            wd = [sbt(p5, "wd%d" % i, [128, 8, D], BF16) for i in range(2)]
            sg = sbt(p5, "sg", [128, 512], F32)
            yrow = [sbt(p5, "yrow%d" % i, [128, D], F32) for i in range(2)]

            def load_exp(k):
                load_w(wg[k % 2], w_eg[k], "wg%d" % (k % 2))
                load_w(wu[k % 2], w_eu[k], "wu%d" % (k % 2))
                load_w(wd[k % 2], w_ed[k], "wd%d" % (k % 2))

            load_w(wd[0], w_ed[0], "wd0")

            yi = 0

            def gather_expert(k):
                for c in range(8):
                    XR = xrow[c]
                    xk = "xrow%d" % c
                    dma("pool", lambda e, XR=XR, k=k, c=c: e.indirect_dma_start(out=XR[:], out_offset=None, in_=Hall,
                                                                               in_offset=bass.IndirectOffsetOnAxis(ap=idxh[:, k, c:c + 1], axis=0)),
                        "g_" + xk, reads=["Hall", "idxh"], writes=[xk])

            def transpose_expert(k):
                for c in range(8):
                    XR = xrow[c]
                    xk = "xrow%d" % c
                    pbt = pb[3 + (c % 2)].bitcast(BF16).rearrange("p (a b) -> p a b", b=128)
                    for dk in range(8):
                        op("pe", lambda e, dk=dk, XR=XR, pbt=pbt: e.transpose(pbt[:, dk, :], XR[:, dk * 128:(dk + 1) * 128], identb[:]), reads=[xk, "identb"], writes=[PB[3 + (c % 2)]], inc=(dk == 7))
                    op("act", lambda e, c=c, pbt=pbt: e.copy(out=xgT[:, :, c * 128:(c + 1) * 128], in_=pbt[:, 0:8, :]), reads=[PB[3 + (c % 2)]], writes=["xgT"])

            gather_expert(0)
            for k in range(4):
                for c in range(8):
                    dma("pool", lambda e, k=k, c=c: e.indirect_dma_start(out=gt4[:, k, c, :], out_offset=None, in_=Gall,
                                                                        in_offset=bass.IndirectOffsetOnAxis(ap=idx[:, k, c:c + 1], axis=0)),
                        "g_gt%d" % k, reads=["Gall", "idx"], writes=["gt4_%d" % k])
            transpose_expert(0)
            for k in range(4):
                if k + 1 < 4:
                    load_exp(k + 1)
                WG, WU, WD = wg[k % 2], wu[k % 2], wd[k % 2]
                kg, ku, kd = "wg%d" % (k % 2), "wu%d" % (k % 2), "wd%d" % (k % 2)
                op("dve", lambda e, k=k: e.tensor_tensor(out=gtm[:], in0=gt4[:, k, :, :], in1=gsel[:, k, :].unsqueeze(1).to_broadcast([128, 8, 16]), op=ALU.mult),
                   reads=["gt4_%d" % k, "gsel"], writes=["gtm"])
                op("dve", lambda e, k=k: e.reduce_sum(out=gate[:, k, :], in_=gtm[:], axis=AX.X), reads=["gtm"], writes=["gate"])
                for st in range(2):
                    ssl = slice(st * 512, (st + 1) * 512)
                    for fc in range(8):
                        fsl = slice(fc * 128, (fc + 1) * 128)
                        bg_, bu_ = (5, 6) if fc % 2 == 0 else (7, 0)
                        mmg(bg_, 512, [(WG[:, dk, fsl], xgT[:, dk, ssl]) for dk in range(8)], [kg, "xgT"])
                        mmg(bu_, 512, [(WU[:, dk, fsl], xgT[:, dk, ssl]) for dk in range(8)], [ku, "xgT"])
                        op("act", lambda e, bg_=bg_: e.activation(out=sg[:], in_=pb[bg_][:], func=AF.Silu), reads=[PB[bg_]], writes=["sg"])
                        op("dve", lambda e, bu_=bu_, fc=fc, ssl=ssl: e.tensor_tensor(out=aT[:, fc, ssl], in0=pb[bu_][:], in1=sg[:], op=ALU.mult), reads=[PB[bu_], "sg"], writes=["aT"])
                if k + 1 < 4:
                    gather_expert(k + 1)
                for c in range(8):
                    yb = yi % 2
                    yi += 1
                    YR = yrow[yb]
                    yk = "yrow%d" % yb
                    csl = slice(c * 128, (c + 1) * 128)
                    for nh in range(2):
                        bk = 1 + nh
                        mmg(bk, 512, [(aT[:, fc, csl], WD[:, fc, nh * 512:(nh + 1) * 512]) for fc in range(8)], [kd, "aT"])
                        op("dve" if nh == 0 else "act",
                           (lambda e, YR=YR, k=k, c=c, bk=bk, nh=nh: e.tensor_scalar(out=YR[:, nh * 512:(nh + 1) * 512], in0=pb[bk][:], scalar1=gate[:, k, c:c + 1], scalar2=None, op0=ALU.mult))
                           if nh == 0 else
                           (lambda e, YR=YR, k=k, c=c, bk=bk, nh=nh: e.mul(out=YR[:, nh * 512:(nh + 1) * 512], in_=pb[bk][:], mul=gate[:, k, c:c + 1])),
                           reads=[PB[bk], "gate"], writes=[yk])
                    dma("pool", lambda e, YR=YR, k=k, c=c: e.indirect_dma_start(out=Dd, out_offset=bass.IndirectOffsetOnAxis(ap=idx[:, k, c:c + 1], axis=0),
                                                                               in_=YR[:], in_offset=None, compute_op=ALU.add),
                        "sc", reads=[yk, "idx", "Dd"], writes=["Dd"])
                if k + 1 < 4:
                    transpose_expert(k + 1)
            if os.environ.get("KSKIP_CC") == "1":
                dma("pool", lambda e: e.dma_start(out=Rr, in_=Dd[0:TOK, :]), "cc", reads=["Dd"], writes=["Rr"])
            else:
                dma("pool", lambda e: e.collective_compute("ReduceScatter", ALU.add, replica_groups=GROUPS, ins=[Dd], outs=[Rr]), "cc3", reads=["Dd"], writes=["Rr"], inc=1)
            if debug:
                dma("sp", lambda e: e.dma_start(out=dbg["R"], in_=Rr), "dbg", reads=["Rr"])
            sch.barrier()
            sch.emit()

        pr.close()
        with ExitStack() as p6:
            gfin = sbt(p6, "gfin", [128, D], F32)
            xr = [sbt(p6, "xr%d" % i, [128, D], F32) for i in range(2)]
            rr = [sbt(p6, "rr%d" % i, [128, D], F32) for i in range(2)]
            zz = [sbt(p6, "zz%d" % i, [128, D], F32) for i in range(2)]
            oo = [sbt(p6, "oo%d" % i, [128, D], F32) for i in range(2)]
            sqj = sbt(p6, "sqj", [128, D], F32)
            ssum = sbt(p6, "ssum", [128, 1], F32)
            dma("sp", lambda e: e.dma_start(out=gfin[:], in_=gfin_d), "c8", writes=["gfin"])
            sqj2 = [sqj, sbt(p6, "sqjb", [128, D], F32)]
            ssum2 = [ssum, sbt(p6, "ssumb", [128, 1], F32)]

            def ld(c):
                b = c % 2
                gsl = slice(c * 128, (c + 1) * 128)
                dma("sp", lambda e, b=b, gsl=gsl: e.dma_start(out=xr[b][:], in_=X2loc[gsl, :]), "ldx%d" % b, reads=["X2loc"], writes=["xr%d" % b])
                dma("sp", lambda e, b=b, gsl=gsl: e.dma_start(out=rr[b][:], in_=Rr[gsl, :]), "ldr%d" % b, reads=["Rr"], writes=["rr%d" % b])
            ld(0)
            for c in range(16):
                b = c % 2
                gsl = slice(c * 128, (c + 1) * 128)
                op("dve", lambda e, b=b: e.tensor_tensor(out=zz[b][:], in0=xr[b][:], in1=rr[b][:], op=ALU.add), reads=["xr%d" % b, "rr%d" % b], writes=["zz%d" % b])
                if c + 1 < 16:
                    ld(c + 1)
                op("act", lambda e, b=b: e.activation(out=sqj2[b][:], in_=zz[b][:], func=AF.Square), reads=["zz%d" % b], writes=["sqj%d" % b])
                op("dve", lambda e, b=b: e.reduce_sum(out=ssum2[b][:], in_=sqj2[b][:], axis=AX.X), reads=["sqj%d" % b], writes=["ssum%d" % b])
                op("act", lambda e, b=b: e.activation(out=ssum2[b][:], in_=ssum2[b][:], func=AF.Sqrt, bias=EPS, scale=1.0 / D), reads=["ssum%d" % b], writes=["ssum%d" % b])
                op("dve", lambda e, b=b: e.reciprocal(out=ssum2[b][:], in_=ssum2[b][:]), reads=["ssum%d" % b], writes=["ssum%d" % b])
                op("dve", lambda e, b=b: e.scalar_tensor_tensor(out=oo[b][:], in0=zz[b][:], scalar=ssum2[b][:, 0:1], in1=gfin[:], op0=ALU.mult, op1=ALU.mult),
                   reads=["zz%d" % b, "ssum%d" % b, "gfin"], writes=["oo%d" % b])
                dma("act", lambda e, b=b, gsl=gsl: e.dma_start(out=out[gsl, :], in_=oo[b][:]), "sto%d" % b, reads=["oo%d" % b])
            sch.barrier()
            sch.emit()
    return nc


def rope_tables(pos0, n):
    inv = 1.0 / (10000.0 ** (np.arange(0, 64, 2, dtype=np.float32) / 64.0))
    ang = (np.arange(pos0, pos0 + n, dtype=np.float32)[:, None] * inv[None, :].astype(np.float32)).astype(np.float32)
    cos = np.cos(ang).astype(np.float32).T
    sin = np.sin(ang).astype(np.float32).T
    cos2 = np.concatenate([cos, cos], 0)
    sin2 = np.concatenate([-sin, sin], 0)
    return np.ascontiguousarray(np.stack([cos2, sin2], 1)).astype(np.float32)


def make_inputs(x, mem, norm_mix_g, w_in, conv_w, conv_b, w_conv_out, q_norm_g, w_uq, kv_norm_g,
                w_ukv, w_mla_out, b_gate, w_mix_out, norm_mem_g, norm_memkv_g, w_mem_q, w_mem_kv,
                w_mem_out, norm_moe_g, w_router, w_exp_gate, w_exp_up, w_exp_down, norm_final_g):
    f = lambda a: np.ascontiguousarray(np.asarray(a, dtype=np.float32))
    x = f(x); mem = f(mem)
    w_in0 = f(w_in[0])
    col = lambda vec: np.ascontiguousarray(f(vec).reshape(-1, 128).T)
    cst = np.concatenate([col(norm_mix_g[0]), col(conv_w[0][0]), col(conv_w[0][1]), col(conv_w[0][2]), col(conv_b[0]),
                          col(q_norm_g[0]), col(kv_norm_g[0]), col(b_gate[0]), col(norm_mem_g[0]), col(norm_memkv_g[0]),
                          col(norm_moe_g[0])], axis=1)
    assert cst.shape == (128, NCST)
    perm = np.concatenate([np.arange(32, 64), np.arange(0, 32)])
    w_krsw = np.ascontiguousarray(w_in0[:, 3456:3520][:, perm])
    wuq = f(w_uq[0])
    w_uqs = np.ascontiguousarray(np.concatenate([wuq[:, h * 192 + 128:h * 192 + 192][:, perm] for h in range(8)], axis=1))
    wukv = f(w_ukv[0])
    w_ukT = np.ascontiguousarray(np.stack([wukv[:, h * 256:h * 256 + 128].T for h in range(8)], axis=1))
    ident = np.eye(128, dtype=np.float32)
    tri = np.triu(np.ones((128, 128), np.float32), 1)
    iot = np.zeros((128, 16), np.float32)
    for c in range(8):
        iot[:, c] = c * 128 + np.arange(128)
    iot[:, 8] = np.arange(128)
    pp = np.arange(128)
    iot[:, 9] = ((pp % 32) // 4) * 16 + (pp // 32) * 4 + (pp % 4)
    iots = np.ascontiguousarray(np.broadcast_to(np.arange(1024, dtype=np.float32)[None, :], (128, 1024)))
    gfin = np.ascontiguousarray(np.broadcast_to(f(norm_final_g)[None, :], (128, D)))
    ropeb = rope_tables(0, S)
    shared = dict(w_in=w_in0, w_krsw=w_krsw, w_conv_out=f(w_conv_out[0]), w_uq=wuq, w_uqs=w_uqs, w_ukT=w_ukT, w_ukv=wukv,
                  w_mla_out=f(w_mla_out[0]), w_mix_out=f(w_mix_out[0]), w_mem_q=f(w_mem_q[0]), w_mem_kv=f(w_mem_kv[0]),
                  w_mem_out=f(w_mem_out[0]), w_router=f(w_router[0]), cst=cst, gfin=gfin, ident=ident, tri=tri, iot=iot,
                  iots=iots, ropeb=ropeb)
    xT = [np.ascontiguousarray(x[b].T) for b in range(2)]
    memT = [np.ascontiguousarray(mem[b].T) for b in range(2)]
    weg, weu, wed = f(w_exp_gate[0]), f(w_exp_up[0]), f(w_exp_down[0])
    in_maps = []
    for c in range(8):
        b, r = c // 4, c % 4
        s0 = r * TOK
        xo = np.zeros((D, TOK + 2), np.float32)
        lo_, hi_ = max(s0 - 1, 0), min(s0 + TOK + 1, S)
        xo[:, lo_ - (s0 - 1):hi_ - (s0 - 1)] = xT[b][:, lo_:hi_]
        p = np.arange(128)
        myrows = np.stack([((p // 32) * 16 + 4 * r + k) * 32 + (p % 32) for k in range(4)], axis=1).astype(np.int32)
        gsel = np.zeros((128, 4, 16), np.float32)
        for k in range(4):
            gsel[:, k, 4 * r + k] = 1.0
        m = dict(shared)
        m.update(xTb=xT[b], xown=xo, memT=memT[b], ropeo=np.ascontiguousarray(ropeb[:, :, s0:s0 + TOK]),
                 w_eg=np.ascontiguousarray(weg[4 * r:4 * r + 4]), w_eu=np.ascontiguousarray(weu[4 * r:4 * r + 4]),
                 w_ed=np.ascontiguousarray(wed[4 * r:4 * r + 4]), myrows=myrows, gsel=gsel)
        in_maps.append(m)
    return in_maps


_NC_CACHE = {}


def run(inputs, debug=False):
    if debug not in _NC_CACHE:
        _NC_CACHE[debug] = build(debug)
    nc = _NC_CACHE[debug]
    in_maps = make_inputs(**inputs)
    res = run_bass_kernel_spmd(nc, in_maps, core_ids=list(range(8)))
    return res


def kernel(**inputs):
    res = run(inputs, debug=False)
    outs = [np.asarray(res.results[c]["out"], dtype=np.float32) for c in range(8)]
    full = np.stack([np.concatenate(outs[0:4], axis=0), np.concatenate(outs[4:8], axis=0)], axis=0)
    return full.astype(np.float32)
```

```python
import os
import numpy as np
from contextlib import ExitStack
import concourse.bass as bass
import concourse.mybir as mybir
from concourse.bass_utils import run_bass_kernel_spmd

F32 = mybir.dt.float32
BF16 = mybir.dt.bfloat16
I32 = mybir.dt.int32
ALU = mybir.AluOpType
AF = mybir.ActivationFunctionType
AX = mybir.AxisListType

D = 1024
S = 8192
TOK = 2048
EPS = 1e-6
NCST = 83


class Sched:
    ENG = ("pe", "act", "dve", "pool", "sp")

    def __init__(self, nc, es):
        self.nc = nc
        self.es = es
        self.ops = {e: [] for e in self.ENG}
        self.sem = {e: es.enter_context(nc.semaphore("s_" + e)) for e in self.ENG}
        self.cnt = {e: 0 for e in self.ENG}
        self.waited = {}
        self.lastw = {}
        self.readers = {}

    def _need(self, eng, tok, waits):
        if tok is None:
            return
        key, val = tok
        if key == eng and eng == "pe":
            return
        if self.waited.get((eng, key), 0) >= val:
            return
        waits[key] = max(waits.get(key, 0), val)

    def _deps(self, eng, reads, writes):
        waits = {}
        for k in reads:
            self._need(eng, self.lastw.get(k), waits)
        for k in writes:
            self._need(eng, self.lastw.get(k), waits)
            for tok in self.readers.get(k, ()):
                self._need(eng, tok, waits)
        for key, val in waits.items():
            self.waited[(eng, key)] = val
            self.ops[eng].append(("w", self.sem[key], val))

    def _commit(self, tok, reads, writes):
        for k in reads:
            self.readers.setdefault(k, []).append(tok)
        for k in writes:
            self.lastw[k] = tok
            self.readers[k] = []

    def op(self, eng, fn, reads=(), writes=(), inc=True):
        self._deps(eng, reads, writes)
        if inc:
            self.cnt[eng] += 1
            tok = (eng, self.cnt[eng])
            self.ops[eng].append(("i", fn, self.sem[eng], 1))
        else:
            tok = (eng, self.cnt[eng] + 1)
            self.ops[eng].append(("n", fn))
        self._commit(tok, reads, writes)
        return tok

    def dma(self, eng, fn, dkey, reads=(), writes=(), inc=16):
        if dkey not in self.sem:
            self.sem[dkey] = self.es.enter_context(self.nc.semaphore("d_" + dkey))
            self.cnt[dkey] = 0
        self._deps(eng, reads, writes)
        self.cnt[dkey] += inc
        tok = (dkey, self.cnt[dkey])
        self.ops[eng].append(("i", fn, self.sem[dkey], inc))
        self._commit(tok, reads, writes)
        return tok

    def barrier(self, skip=()):
        for eng in self.ENG:
            for key, val in self.cnt.items():
                if key == eng or val == 0 or key in skip:
                    continue
                if self.waited.get((eng, key), 0) >= val:
                    continue
                self.waited[(eng, key)] = val
                self.ops[eng].append(("w", self.sem[key], val))

    def emit(self):
        nc = self.nc
        ops = self.ops
        self.ops = {e: [] for e in self.ENG}
        with nc.Block() as block:
            def run(engname):
                def body(e):
                    for o in ops[engname]:
                        if o[0] == "w":
                            e.wait_ge(o[1], o[2])
                        elif o[0] == "n":
                            o[1](e)
                        else:
                            o[1](e).then_inc(o[2], o[3])
                return body
            block.tensor(run("pe"))
            block.scalar(run("act"))
            block.vector(run("dve"))
            block.gpsimd(run("pool"))
            block.sync(run("sp"))


def bc(ap2, n):
    return ap2.unsqueeze(2).to_broadcast([ap2.shape[0], ap2.shape[1], n])


def build(debug=False):
    nc = bass.Bass("TRN2", target_bir_lowering=False)
    di = {}

    def inp(name, shape, dt=F32):
        di[name] = nc.dram_tensor(name, shape, dt, kind="ExternalInput").ap()
        return di[name]

    xTb = inp("xTb", [D, S])
    xown = inp("xown", [D, TOK + 2])
    memT = inp("memT", [D, 256])
    w_in = inp("w_in", [D, 5568])
    w_krsw = inp("w_krsw", [D, 64])
    w_conv_out = inp("w_conv_out", [D, D])
    w_uq = inp("w_uq", [256, 1536])
    w_uqs = inp("w_uqs", [256, 512])
    w_ukT = inp("w_ukT", [128, 8, 128])
    w_ukv = inp("w_ukv", [128, 2048])
    w_mla_out = inp("w_mla_out", [D, D])
    w_mix_out = inp("w_mix_out", [D, D])
    w_mem_q = inp("w_mem_q", [D, D])
    w_mem_kv = inp("w_mem_kv", [D, 2048])
    w_mem_out = inp("w_mem_out", [D, D])
    w_router = inp("w_router", [D, 16])
    w_eg = inp("w_eg", [4, D, D])
    w_eu = inp("w_eu", [4, D, D])
    w_ed = inp("w_ed", [4, D, D])
    cst_d = inp("cst", [128, NCST])
    gfin_d = inp("gfin", [128, D])
    ident_d = inp("ident", [128, 128])
    tri_d = inp("tri", [128, 128])
    iot_d = inp("iot", [128, 16])
    iots_d = inp("iots", [128, 1024])
    ropeb = inp("ropeb", [64, 2, S])
    ropeo = inp("ropeo", [64, 2, TOK])
    myrows_d = inp("myrows", [128, 4], I32)
    gsel_d = inp("gsel", [128, 4, 16])
    out = nc.dram_tensor("out", [TOK, D], F32, kind="ExternalOutput").ap()
    dbg = {}
    if debug:
        dbg["x1"] = nc.dram_tensor("dbg_x1", [D, TOK], F32, kind="ExternalOutput").ap()
        dbg["x2"] = nc.dram_tensor("dbg_x2", [TOK, D], F32, kind="ExternalOutput").ap()
        dbg["R"] = nc.dram_tensor("dbg_R", [TOK, D], F32, kind="ExternalOutput").ap()
        dbg["idx"] = nc.dram_tensor("dbg_idx", [128, 32], I32, kind="ExternalOutput").ap()
        dbg["thr"] = nc.dram_tensor("dbg_thr", [128, 4], F32, kind="ExternalOutput").ap()

    Hloc = nc.dram_tensor("Hloc", [TOK, 1024], BF16).ap()
    Hall = nc.dram_tensor("Hall", [4 * TOK, 1024], BF16).ap()
    Gloc = nc.dram_tensor("Gloc", [TOK, 16], F32).ap()
    Gall = nc.dram_tensor("Gall", [4 * TOK, 16], F32).ap()
    Aloc = nc.dram_tensor("Aloc", [16, TOK], F32).ap()
    Aall = nc.dram_tensor("Aall", [64, TOK], F32).ap()
    X2loc = nc.dram_tensor("X2loc", [TOK, D], F32).ap()
    Dd = nc.dram_tensor("Dd", [4 * TOK, D], F32).ap()
    Rr = nc.dram_tensor("Rr", [TOK, D], F32).ap()
    GROUPS = [[0, 1, 2, 3], [4, 5, 6, 7]]

    with ExitStack() as es:
        sch = Sched(nc, es)
        op, dma = sch.op, sch.dma

        def sbt(stack, name, shape, dt):
            return stack.enter_context(nc.sbuf_tensor("sb_" + name, shape, dt))

        pbig = es.enter_context(nc.psum_tensor("pbig", [128, 8, 512], F32))
        pb = [pbig[:, i, :] for i in range(8)]
        PB = ["pb%d" % i for i in range(8)]

        cst = sbt(es, "cst", [128, NCST], F32)
        onesb = sbt(es, "onesb", [128, 128], BF16)
        identb = sbt(es, "identb", [128, 128], BF16)
        identf = sbt(es, "identf", [128, 128], F32)
        rstd_own = sbt(es, "rstd_own", [128, TOK + 2], F32)
        zt = sbt(es, "zt", [128, 2048], F32)
        dma("sp", lambda e: e.dma_start(out=cst[:], in_=cst_d), "c0", writes=["cst"])
        dma("sp", lambda e: e.dma_start(out=identf[:], in_=ident_d), "c1", writes=["identf"])
        dma("pool", lambda e: e.dma_start(out=identb[:], in_=ident_d), "c2", writes=["identb"])
        op("dve", lambda e: e.memset(onesb[:], 1.0), writes=["onesb"])
        GMIX = cst[:, 0:8]
        CW = [cst[:, 8:16], cst[:, 16:24], cst[:, 24:32]]
        CB = cst[:, 32:40]
        GQ = cst[:, 40:42]
        GKV = cst[:, 42:43]
        BG = cst[:, 43:59]
        GMEM = cst[:, 59:67]
        GMKV = cst[:, 67:75]
        GMOE = cst[:, 75:83]

        def load_w(stack_t, src_ap, key, fold=False, eng="pool"):
            dma("pool", lambda e: e.dma_start(out=stack_t[:], in_=src_ap.rearrange("(k p) c -> p k c", p=128)), "w_" + key, writes=[key])
            if fold:
                n = stack_t.shape[2]
                op(eng, lambda e: e.tensor_tensor(out=stack_t[:], in0=stack_t[:], in1=bc(GMIX, n), op=ALU.mult), reads=[key, "cst"], writes=[key])

        def rms_rstd(xsq_ap_fn, nk, n, out_ap, keys_r, key_w, inv_dim, bank=0, tmpkey="rt"):
            for k in range(nk):
                op("pe", lambda e, k=k: e.matmul(pb[bank][:, 0:n], lhsT=onesb[:], rhs=xsq_ap_fn(k), start=(k == 0), stop=(k == nk - 1)),
                   reads=["onesb"] + keys_r, writes=[PB[bank]], inc=(k == nk - 1))
            op("act", lambda e: e.activation(out=out_ap, in_=pb[bank][:, 0:n], func=AF.Ln, bias=EPS, scale=inv_dim), reads=[PB[bank]], writes=[key_w])
            op("act", lambda e: e.activation(out=out_ap, in_=out_ap, func=AF.Exp, scale=-0.5), reads=[key_w], writes=[key_w])

        def mmg(bank, n, pairs, reads, m=128, col0=0):
            L = len(pairs)
            for i, (l, r) in enumerate(pairs):
                op("pe", lambda e, i=i, l=l, r=r: e.matmul(pb[bank][0:m, col0:col0 + n], lhsT=l, rhs=r, start=(i == 0), stop=(i == L - 1)),
                   reads=reads, writes=[PB[bank]], inc=(i == L - 1))

        TOP = nc._sbuf_addr_for_side("right")
        attn = nc.alloc_sbuf_tensor_at("sb_attn", [128, 8, TOK], BF16, offset=TOP - 32768)
        mixed = nc.alloc_sbuf_tensor_at("sb_mixed", [128, 8, TOK], BF16, offset=TOP - 98304)
        x1 = nc.alloc_sbuf_tensor_at("sb_x1", [128, 8, TOK], F32, offset=TOP - 65536)
        with ExitStack() as p1:
            Kn = sbt(p1, "Kn", [128, S], BF16)
            Kr = sbt(p1, "Kr", [128, S], BF16)
            V = sbt(p1, "V", [128, 64, 128], BF16)
            w1 = sbt(p1, "w1", [128, 8, 256], BF16)
            wcq = sbt(p1, "wcq", [128, 8, 256], BF16)
            wuq = sbt(p1, "wuq", [128, 2, 1536], BF16)
            wuqs = sbt(p1, "wuqs", [128, 2, 512], BF16)
            wukT = sbt(p1, "wukT", [128, 8, 128], BF16)
            wuv = sbt(p1, "wuv", [128, 8, 128], BF16)
            xbt = [sbt(p1, "xbt%d" % i, [128, 8, 512], BF16) for i in range(2)]
            xsq = sbt(p1, "xsq", [128, 8, 512], BF16)
            cs = [sbt(p1, "cs%d" % i, [64, 2, 512], F32) for i in range(2)]
            rstd = sbt(p1, "rstd", [128, 512], F32)
            ckv32 = sbt(p1, "ckv32", [128, 512], F32)
            sq2 = sbt(p1, "sq2", [128, 2, 512], BF16)
            r2 = sbt(p1, "r2", [128, 512], F32)
            t1 = sbt(p1, "t1", [64, 512], F32)
            t2 = sbt(p1, "t2", [64, 512], F32)
            cq32 = sbt(p1, "cq32", [128, 2, 512], F32)
            cqn = sbt(p1, "cqn", [128, 2, 512], BF16)
            qn = sbt(p1, "qn", [128, 512], BF16)
            qabs = sbt(p1, "qabs", [128, 8, 512], BF16)
            qrope = sbt(p1, "qrope", [128, 8, 512], BF16)
            Pp = [sbt(p1, "Pp%d" % i, [128, 2, 512], BF16) for i in range(3)]
            Ps2 = [sbt(p1, "Ps2_%d" % i, [128, 512], BF16) for i in range(2)]
            rD = sbt(p1, "rD", [128, 512], F32)
            pc = sbt(p1, "pc", [128, 512], BF16)

            op("pool", lambda e: e.memset(Kr[64:128, :], 0.0), writes=["Krz"])
            op("pool", lambda e: e.memset(qrope[64:128, :, :], 0.0), writes=["qropez"])
            dma("pool", lambda e: e.dma_start(out=w1[:, :, 0:192], in_=w_in[:, 3328:3520].rearrange("(k p) c -> p k c", p=128)), "w_w1a", writes=["w1"])
            dma("pool", lambda e: e.dma_start(out=w1[:, :, 192:256], in_=w_krsw.rearrange("(k p) c -> p k c", p=128)), "w_w1b", writes=["w1"])
            op("pool", lambda e: e.tensor_tensor(out=w1[:], in0=w1[:], in1=bc(GMIX, 256), op=ALU.mult), reads=["w1", "cst"], writes=["w1"])
            load_w(wcq, w_in[:, 3072:3328], "wcq", fold=True)
            dma("pool", lambda e: e.dma_start(out=wuq[:], in_=w_uq.rearrange("(k p) c -> p k c", p=128)), "w_wuq", writes=["wuq"])
            dma("pool", lambda e: e.dma_start(out=wuqs[:], in_=w_uqs.rearrange("(k p) c -> p k c", p=128)), "w_wuqs", writes=["wuqs"])
            dma("pool", lambda e: e.dma_start(out=wukT[:], in_=w_ukT), "w_wukT", writes=["wukT"])
            dma("pool", lambda e: e.dma_start(out=wuv[:], in_=w_ukv.rearrange("l (h c) -> l h c", c=256)[:, :, 128:256]), "w_wuv", writes=["wuv"])

            def load_tile(i, src, col0, rope_src, rcol0):
                b = i % 2
                dma("pool", lambda e: e.dma_start(out=xbt[b][:], in_=src[:, col0:col0 + 512].rearrange("(k p) t -> p k t", p=128)), "xbt%d" % b, writes=["xbt%d" % b])
                dma("sp", lambda e: e.dma_start(out=cs[b][:], in_=rope_src[:, :, rcol0:rcol0 + 512]), "cs%d" % b, writes=["cs%d" % b])

            def tile_rstd(b, dst_ap, dst_key):
                op("act", lambda e: e.activation(out=xsq[:], in_=xbt[b][:], func=AF.Square), reads=["xbt%d" % b], writes=["xsq"])
                rms_rstd(lambda k: xsq[:, k, :], 8, 512, dst_ap, ["xsq"], dst_key, 1.0 / D, bank=0)

            def rope_to(dst_ap, dst_key, bank_a, bank_b, b, rs_ap=None, rs_key=None):
                op("dve", lambda e: e.tensor_tensor(out=t1[:], in0=pb[bank_a][0:64, :], in1=cs[b][:, 0, :], op=ALU.mult), reads=[PB[bank_a], "cs%d" % b], writes=["t1"])
                op("dve", lambda e: e.tensor_tensor(out=t2[:], in0=pb[bank_b][0:64, :], in1=cs[b][:, 1, :], op=ALU.mult), reads=[PB[bank_b], "cs%d" % b], writes=["t2"])
                if rs_ap is None:
                    op("dve", lambda e: e.tensor_tensor(out=dst_ap, in0=t1[:], in1=t2[:], op=ALU.add), reads=["t1", "t2"], writes=[dst_key])
                else:
                    op("dve", lambda e: e.tensor_tensor(out=t1[:], in0=t1[:], in1=t2[:], op=ALU.add), reads=["t1", "t2"], writes=["t1"])
                    op("dve", lambda e: e.tensor_tensor(out=dst_ap, in0=t1[:], in1=rs_ap, op=ALU.mult), reads=["t1", rs_key], writes=[dst_key])

            load_tile(0, xTb, 0, ropeb, 0)
            for i in range(16):
                b = i % 2
                if i + 1 < 16:
                    load_tile(i + 1, xTb, (i + 1) * 512, ropeb, (i + 1) * 512)
                tsl = slice(i * 512, (i + 1) * 512)
                tile_rstd(b, rstd[:], "rstd")
                xk = "xbt%d" % b
                mmg(1, 512, [(w1[:, k, 0:128], xbt[b][:, k, :]) for k in range(8)], [xk, "w1"])
                mmg(2, 512, [(w1[:, k, 128:192], xbt[b][:, k, :]) for k in range(8)], [xk, "w1"], m=64)
                mmg(3, 512, [(w1[:, k, 192:256], xbt[b][:, k, :]) for k in range(8)], [xk, "w1"], m=64)
                op("dve", lambda e: e.tensor_tensor(out=ckv32[:], in0=pb[1][:], in1=rstd[:], op=ALU.mult), reads=[PB[1], "rstd"], writes=["ckv32"])
                op("act", lambda e: e.activation(out=sq2[:, 0, :], in_=ckv32[:], func=AF.Square), reads=["ckv32"], writes=["sq2"])
                rms_rstd(lambda k: sq2[:, 0, :], 1, 512, r2[:], ["sq2"], "r2", 1.0 / 128, bank=4)
                op("dve", lambda e, tsl=tsl: e.scalar_tensor_tensor(out=Kn[:, tsl], in0=ckv32[:], scalar=GKV[:, 0:1], in1=r2[:], op0=ALU.mult, op1=ALU.mult),
                   reads=["ckv32", "r2", "cst"], writes=["Kn%d" % i])
                rope_to(Kr[0:64, tsl], "Kr%d" % i, 2, 3, b, rs_ap=rstd[0:64, :], rs_key="rstd")
                pbv = pb[5][:].bitcast(BF16).rearrange("p (a b) -> p a b", b=128)
                for a in range(4):
                    op("pe", lambda e, a=a, i=i: e.transpose(pbv[:, a, :], Kn[:, i * 512 + a * 128:i * 512 + (a + 1) * 128], identb[:]),
                       reads=["Kn%d" % i, "identb"], writes=[PB[5]], inc=(a == 3))
                op("act", lambda e, i=i: e.copy(out=V[:, 4 * i:4 * i + 4, :], in_=pbv[:, 0:4, :]), reads=[PB[5]], writes=["V%d" % i])

            KV_KEYS = ["Kn%d" % i for i in range(16)] + ["Kr%d" % i for i in range(16)] + ["V%d" % i for i in range(16)]
            SCALE = float(192 ** -0.5)

            for t in range(4):
                b = t % 2
                load_tile(t, xown, 1 + t * 512, ropeo, t * 512)
                osl = slice(1 + t * 512, 1 + (t + 1) * 512)
                tile_rstd(b, rstd_own[:, osl], "rstd_own")
                xk = "xbt%d" % b
                for c in range(2):
                    mmg(1 + c, 512, [(wcq[:, k, c * 128:(c + 1) * 128], xbt[b][:, k, :]) for k in range(8)], [xk, "wcq"])
                    op("dve", lambda e, c=c, osl=osl: e.tensor_tensor(out=cq32[:, c, :], in0=pb[1 + c][:], in1=rstd_own[:, osl], op=ALU.mult),
                       reads=[PB[1 + c], "rstd_own"], writes=["cq32"])
                op("act", lambda e: e.activation(out=sq2[:], in_=cq32[:], func=AF.Square), reads=["cq32"], writes=["sq2"])
                rms_rstd(lambda k: sq2[:, k, :], 2, 512, r2[:], ["sq2"], "r2", 1.0 / 256, bank=4)
                for c in range(2):
                    op("dve", lambda e, c=c: e.scalar_tensor_tensor(out=cqn[:, c, :], in0=cq32[:, c, :], scalar=GQ[:, c:c + 1], in1=r2[:], op0=ALU.mult, op1=ALU.mult),
                       reads=["cq32", "r2", "cst"], writes=["cqn"])
                for h in range(8):
                    mmg(5, 512, [(wuq[:, c, h * 192:h * 192 + 128], cqn[:, c, :]) for c in range(2)], ["wuq", "cqn"])
                    op("act", lambda e: e.copy(out=qn[:], in_=pb[5][:]), reads=[PB[5]], writes=["qn"])
                    mmg(6, 512, [(wukT[:, h, :], qn[:])], ["wukT", "qn"])
                    op("act", lambda e, h=h: e.copy(out=qabs[:, h, :], in_=pb[6][:]), reads=[PB[6]], writes=["qabs"])
                    mmg(3, 512, [(wuq[:, c, h * 192 + 128:h * 192 + 192], cqn[:, c, :]) for c in range(2)], ["wuq", "cqn"], m=64)
                    mmg(7, 512, [(wuqs[:, c, h * 64:(h + 1) * 64], cqn[:, c, :]) for c in range(2)], ["wuqs", "cqn"], m=64)
                    rope_to(qrope[0:64, h, :], "qrope", 3, 7, b)
                SPAIR = [(0, 1), (2, 3)]
                for h in range(8):
                    def s_pair(j, h=h):
                        for u in range(2):
                            kc = 2 * j + u
                            bk = SPAIR[j % 2][u]
                            ksl = slice(kc * 128, (kc + 1) * 128)
                            op("pe", lambda e, bk=bk, ksl=ksl: e.matmul(pb[bk], lhsT=Kn[:, ksl], rhs=qabs[:, h, :], start=True, stop=False),
                               reads=["Kn%d" % (kc // 4), "qabs"], writes=[PB[bk]], inc=False)
                            op("pe", lambda e, bk=bk, ksl=ksl: e.matmul(pb[bk], lhsT=Kr[:, ksl], rhs=qrope[:, h, :], start=False, stop=True),
                               reads=["Kr%d" % (kc // 4), "qrope", "Krz", "qropez"], writes=[PB[bk]], inc=(u == 1))

                    s_pair(0)
                    for j in range(32):
                        if j + 1 < 32:
                            s_pair(j + 1)
                        b0, b1 = SPAIR[j % 2]
                        P = Pp[j % 3]
                        pk = "Pp%d" % (j % 3)
                        op("act", lambda e, P=P, b0=b0: e.activation(out=P[:], in_=pbig[:, b0:b0 + 2, :], func=AF.Exp, scale=SCALE), reads=[PB[b0], PB[b1]], writes=[pk])
                        for u in range(2):
                            kc = 2 * j + u
                            op("pe", lambda e, P=P, kc=kc, u=u: e.matmul(pb[4], lhsT=V[:, kc, :], rhs=P[:, u, :], start=(kc == 0), stop=(kc == 63)),
                               reads=["V%d" % (kc // 4), pk], writes=[PB[4]], inc=False)
                        for u in range(2):
                            kc = 2 * j + u
                            op("pe", lambda e, P=P, kc=kc, u=u: e.matmul(pb[5], lhsT=onesb[:], rhs=P[:, u, :], start=(kc == 0), stop=(kc == 63)),
                               reads=["onesb", pk], writes=[PB[5]], inc=(u == 1))
                    op("dve", lambda e: e.reciprocal(out=rD[:], in_=pb[5]), reads=[PB[5]], writes=["rD"])
                    op("dve", lambda e: e.tensor_tensor(out=pc[:], in0=pb[4], in1=rD[:], op=ALU.mult), reads=[PB[4], "rD"], writes=["pc"])
                    mmg(6, 512, [(wuv[:, h, :], pc[:])], ["wuv", "pc"])
                    op("act", lambda e, h=h, t=t: e.copy(out=attn[:, h, t * 512:(t + 1) * 512], in_=pb[6]), reads=[PB[6]], writes=["attn"])
            sch.barrier()
            sch.emit()

        PIECES = [(0, 512), (512, 512), (1024, 512), (1536, 512), (2048, 2)]
        with ExitStack() as p2:
            xbo = sbt(p2, "xbo", [128, 8, TOK + 2], BF16)
            ycin = sbt(p2, "ycin", [128, 8, TOK], BF16)
            ta = sbt(p2, "ta", [128, 512], F32)
            tb = sbt(p2, "tb", [128, 512], F32)
            hsq = sbt(p2, "hsq", [128, 8, 2], BF16)
            p2c = ExitStack()
            wc = [sbt(p2c, "wc%d" % i, [128, 8, 384], BF16) for i in range(2)]
            v = sbt(p2c, "v", [128, TOK + 2], F32)
            gbv = sbt(p2c, "gbv", [128, TOK + 2], F32)
            c1 = sbt(p2c, "c1", [128, TOK], F32)
            c2 = sbt(p2c, "c2", [128, TOK], F32)

            dma("pool", lambda e: e.dma_start(out=xbo[:], in_=xown.rearrange("(k p) t -> p k t", p=128)), "xbo", writes=["xbo"])
            for hc, col in enumerate((0, TOK + 1)):
                op("act", lambda e, hc=hc, col=col: e.activation(out=hsq[:, :, hc:hc + 1], in_=xbo[:, :, col:col + 1], func=AF.Square), reads=["xbo"], writes=["hsq"])
            for hc, col in enumerate((0, TOK + 1)):
                rms_rstd(lambda k, hc=hc: hsq[:, k, hc:hc + 1], 8, 1, rstd_own[:, col:col + 1], ["hsq"], "rstd_own", 1.0 / D, bank=0)

            def load_wc(j):
                t_ = wc[j % 2]
                key = "wc%d" % (j % 2)
                for g_ in range(3):
                    dma("pool", lambda e, g_=g_: e.dma_start(out=t_[:, :, g_ * 128:(g_ + 1) * 128],
                                                          in_=w_in[:, g_ * 1024 + j * 128:g_ * 1024 + (j + 1) * 128].rearrange("(k p) c -> p k c", p=128)),
                        "w_" + key, writes=[key])
                op("pool", lambda e: e.tensor_tensor(out=t_[:], in0=t_[:], in1=bc(GMIX, 384), op=ALU.mult), reads=[key, "cst"], writes=[key])

            load_wc(0)
            for j in range(8):
                if j + 1 < 8:
                    load_wc(j + 1)
                W = wc[j % 2]
                wk = "wc%d" % (j % 2)
                for (c0, n) in PIECES:
                    psl = slice(c0, c0 + n)
                    mmg(1, n, [(W[:, k, 0:128], xbo[:, k, psl]) for k in range(8)], [wk, "xbo"])
                    mmg(2, n, [(W[:, k, 128:256], xbo[:, k, psl]) for k in range(8)], [wk, "xbo"])
                    mmg(3, n, [(W[:, k, 256:384], xbo[:, k, psl]) for k in range(8)], [wk, "xbo"])
                    op("dve", lambda e, n=n, psl=psl: e.tensor_tensor(out=ta[:, 0:n], in0=pb[1][:, 0:n], in1=rstd_own[:, psl], op=ALU.mult), reads=[PB[1], "rstd_own"], writes=["ta"])
                    op("dve", lambda e, n=n, psl=psl: e.tensor_tensor(out=tb[:, 0:n], in0=pb[3][:, 0:n], in1=rstd_own[:, psl], op=ALU.mult), reads=[PB[3], "rstd_own"], writes=["tb"])
                    op("dve", lambda e, n=n, psl=psl: e.tensor_tensor(out=v[:, psl], in0=ta[:, 0:n], in1=tb[:, 0:n], op=ALU.mult), reads=["ta", "tb"], writes=["v"])
                    op("dve", lambda e, n=n, psl=psl: e.tensor_tensor(out=gbv[:, psl], in0=pb[2][:, 0:n], in1=rstd_own[:, psl], op=ALU.mult), reads=[PB[2], "rstd_own"], writes=["gbv"])
                op("pool", lambda e, j=j: e.tensor_scalar(out=c1[:], in0=v[:, 0:TOK], scalar1=CW[0][:, j:j + 1], scalar2=CB[:, j:j + 1], op0=ALU.mult, op1=ALU.add),
                   reads=["v", "cst"], writes=["c1"])
                op("dve", lambda e, j=j: e.scalar_tensor_tensor(out=c2[:], in0=v[:, 1:TOK + 1], scalar=CW[1][:, j:j + 1], in1=c1[:], op0=ALU.mult, op1=ALU.add),
                   reads=["v", "c1", "cst"], writes=["c2"])
                op("dve", lambda e, j=j: e.scalar_tensor_tensor(out=c1[:], in0=v[:, 2:TOK + 2], scalar=CW[2][:, j:j + 1], in1=c2[:], op0=ALU.mult, op1=ALU.add),
                   reads=["v", "c2", "cst"], writes=["c1"])
                op("pool", lambda e, j=j: e.tensor_tensor(out=ycin[:, j, :], in0=c1[:], in1=gbv[:, 1:TOK + 1], op=ALU.mult), reads=["c1", "gbv"], writes=["ycin"])

            sch.barrier()
            sch.emit()
            p2c.close()
            wm = [sbt(p2, "wm%d" % i, [128, 8, 512], BF16) for i in range(2)]
            sga = sbt(p2, "sga", [128, 512], F32)
            sgb = sbt(p2, "sgb", [128, 512], F32)

            def load_wm(j):
                t_ = wm[j % 2]
                key = "wm%d" % (j % 2)
                jsl = slice(j * 128, (j + 1) * 128)
                dma("pool", lambda e: e.dma_start(out=t_[:, :, 0:128], in_=w_conv_out[:, jsl].rearrange("(k p) c -> p k c", p=128)), "w_" + key, writes=[key])
                dma("pool", lambda e: e.dma_start(out=t_[:, :, 128:256], in_=w_mla_out[:, jsl].rearrange("(k p) c -> p k c", p=128)), "w_" + key, writes=[key])
                dma("pool", lambda e: e.dma_start(out=t_[:, :, 256:384], in_=w_in[:, 3520 + j * 128:3520 + (j + 1) * 128].rearrange("(k p) c -> p k c", p=128)), "w_" + key, writes=[key])
                dma("pool", lambda e: e.dma_start(out=t_[:, :, 384:512], in_=w_in[:, 4544 + j * 128:4544 + (j + 1) * 128].rearrange("(k p) c -> p k c", p=128)), "w_" + key, writes=[key])
                op("pool", lambda e: e.tensor_tensor(out=t_[:, :, 256:512], in0=t_[:, :, 256:512], in1=bc(GMIX, 256), op=ALU.mult), reads=[key, "cst"], writes=[key])

            load_wm(0)
            for j in range(8):
                if j + 1 < 8:
                    load_wm(j + 1)
                W = wm[j % 2]
                wk = "wm%d" % (j % 2)
                for t in range(4):
                    tsl = slice(t * 512, (t + 1) * 512)
                    osl = slice(1 + t * 512, 1 + (t + 1) * 512)
                    mmg(1, 512, [(W[:, k, 0:128], ycin[:, k, tsl]) for k in range(8)], [wk, "ycin"])
                    mmg(2, 512, [(W[:, k, 128:256], attn[:, k, tsl]) for k in range(8)], [wk, "attn"])
                    mmg(3, 512, [(W[:, k, 256:384], xbo[:, k, osl]) for k in range(8)], [wk, "xbo"])
                    mmg(4, 512, [(W[:, k, 384:512], xbo[:, k, osl]) for k in range(8)], [wk, "xbo"])
                    op("dve", lambda e, osl=osl: e.tensor_tensor(out=ta[:], in0=pb[3][:], in1=rstd_own[:, osl], op=ALU.mult), reads=[PB[3], "rstd_own"], writes=["ta"])
                    op("act", lambda e, j=j: e.activation(out=sga[:], in_=ta[:], func=AF.Sigmoid, bias=BG[:, j:j + 1]), reads=["ta", "cst"], writes=["sga"])
                    op("dve", lambda e, osl=osl: e.tensor_tensor(out=tb[:], in0=pb[4][:], in1=rstd_own[:, osl], op=ALU.mult), reads=[PB[4], "rstd_own"], writes=["tb"])
                    op("act", lambda e, j=j: e.activation(out=sgb[:], in_=tb[:], func=AF.Sigmoid, bias=BG[:, 8 + j:9 + j]), reads=["tb", "cst"], writes=["sgb"])
                    op("dve", lambda e: e.tensor_tensor(out=sga[:], in0=pb[1][:], in1=sga[:], op=ALU.mult), reads=[PB[1], "sga"], writes=["sga"])
                    op("dve", lambda e: e.tensor_tensor(out=sgb[:], in0=pb[2][:], in1=sgb[:], op=ALU.mult), reads=[PB[2], "sgb"], writes=["sgb"])
                    op("dve", lambda e, j=j, tsl=tsl: e.tensor_tensor(out=mixed[:, j, tsl], in0=sga[:], in1=sgb[:], op=ALU.add), reads=["sga", "sgb"], writes=["mixed"])
            sch.barrier()
            sch.emit()

        with ExitStack() as p2b:
            wx = [sbt(p2b, "wx%d" % i, [128, 8, 128], BF16) for i in range(2)]
            dma("sp", lambda e: e.dma_start(out=x1[:], in_=xown[:, 1:TOK + 1].rearrange("(k p) t -> p k t", p=128)), "x1ld", writes=["x1"])

            def load_wx(j):
                load_w(wx[j % 2], w_mix_out[:, j * 128:(j + 1) * 128], "wx%d" % (j % 2))
            load_wx(0)
            for j in range(8):
                if j + 1 < 8:
                    load_wx(j + 1)
                for t in range(4):
                    tsl = slice(t * 512, (t + 1) * 512)
                    bk = 1 + (t % 2)
                    mmg(bk, 512, [(wx[j % 2][:, k, :], mixed[:, k, tsl]) for k in range(8)], ["wx%d" % (j % 2), "mixed"])
                    op("dve", lambda e, j=j, tsl=tsl, bk=bk: e.tensor_tensor(out=x1[:, j, tsl], in0=pb[bk][:], in1=x1[:, j, tsl], op=ALU.add), reads=[PB[bk], "x1"], writes=["x1"])
            if debug:
                dma("sp", lambda e: e.dma_start(out=dbg["x1"].rearrange("(k p) t -> p k t", p=128), in_=x1[:]), "dbg", reads=["x1"])
            op("pool", lambda e: e.memset(zt[:], 0.0), writes=["zt"])
            for z in range(32):
                dma("sp", lambda e, z=z: e.dma_start(out=Dd[z * 256:(z + 1) * 256, :].rearrange("(p a) d -> p (a d)", a=2), in_=zt[:]), "zD", reads=["zt"])
            sch.barrier(skip=("zD",))
            sch.emit()

        with ExitStack() as p3:
            K2T = sbt(p3, "K2T", [128, 8, 256], BF16)
            V2 = sbt(p3, "V2", [128, 2, D], BF16)
            hsq3 = sbt(p3, "hsq3", [128, 8, 512], BF16)
            rs3 = sbt(p3, "rs3", [128, 512], F32)
            SC2 = float(256 ** -0.5)
            w2q = sbt(p3, "w2q", [128, 8, D], BF16)
            w2o = sbt(p3, "w2o", [128, 8, D], BF16)
            with ExitStack() as p3a:
                wkv = sbt(p3a, "wkv", [128, 8, 2048], BF16)
                mem32 = sbt(p3a, "mem32", [128, 8, 256], F32)
                memn = sbt(p3a, "memn", [128, 8, 256], BF16)
                load_w(wkv, w_mem_kv, "wkv")
                load_w(w2q, w_mem_q, "w2q")
                load_w(w2o, w_mem_out, "w2o")
                dma("sp", lambda e: e.dma_start(out=mem32[:], in_=memT.rearrange("(k p) t -> p k t", p=128)), "mem", writes=["mem32"])
                op("act", lambda e: e.activation(out=hsq3[:, :, 0:256], in_=mem32[:], func=AF.Square), reads=["mem32"], writes=["hsq3"])
                rms_rstd(lambda k: hsq3[:, k, 0:256], 8, 256, rs3[:, 0:256], ["hsq3"], "rs3", 1.0 / D, bank=0)
                for k in range(8):
                    op("dve", lambda e, k=k: e.scalar_tensor_tensor(out=memn[:, k, :], in0=mem32[:, k, :], scalar=GMKV[:, k:k + 1], in1=rs3[:, 0:256], op0=ALU.mult, op1=ALU.mult),
                       reads=["mem32", "rs3", "cst"], writes=["memn"])
                for m in range(8):
                    bk = 1 + (m % 2)
                    mmg(bk, 256, [(wkv[:, k, m * 128:(m + 1) * 128], memn[:, k, :]) for k in range(8)], ["wkv", "memn"])
                    op("act", lambda e, m=m, bk=bk: e.copy(out=K2T[:, m, :], in_=pb[bk][:, 0:256]), reads=[PB[bk]], writes=["K2T"])
                for kc in range(2):
                    for nh in range(2):
                        bk = 3 + nh
                        mmg(bk, 512, [(memn[:, k, kc * 128:(kc + 1) * 128], wkv[:, k, 1024 + nh * 512:1024 + (nh + 1) * 512]) for k in range(8)], ["wkv", "memn"])
                        op("act", lambda e, kc=kc, nh=nh, bk=bk: e.copy(out=V2[:, kc, nh * 512:(nh + 1) * 512], in_=pb[bk][:]), reads=[PB[bk]], writes=["V2"])
                sch.barrier(skip=("zD",))
                sch.emit()
            h2 = sbt(p3, "h2", [128, 8, 512], BF16)
            q2 = sbt(p3, "q2", [128, 8, 512], BF16)
            o2 = sbt(p3, "o2", [128, 8, 512], BF16)
            P2 = [sbt(p3, "P2_%d" % i, [128, 512], BF16) for i in range(2)]
            rD2 = sbt(p3, "rD2", [128, 512], F32)

            for t in range(4):
                tsl = slice(t * 512, (t + 1) * 512)
                op("act", lambda e, tsl=tsl: e.activation(out=hsq3[:], in_=x1[:, :, tsl], func=AF.Square), reads=["x1"], writes=["hsq3"])
                rms_rstd(lambda k: hsq3[:, k, :], 8, 512, rs3[:], ["hsq3"], "rs3", 1.0 / D, bank=0)
                for k in range(8):
                    op("dve", lambda e, k=k, tsl=tsl: e.scalar_tensor_tensor(out=h2[:, k, :], in0=x1[:, k, tsl], scalar=GMEM[:, k:k + 1], in1=rs3[:], op0=ALU.mult, op1=ALU.mult),
                       reads=["x1", "rs3", "cst"], writes=["h2"])
                for m in range(8):
                    bk = 1 + (m % 2)
                    mmg(bk, 512, [(w2q[:, k, m * 128:(m + 1) * 128], h2[:, k, :]) for k in range(8)], ["w2q", "h2"])
                    op("act", lambda e, m=m, bk=bk: e.copy(out=q2[:, m, :], in_=pb[bk][:]), reads=[PB[bk]], writes=["q2"])
                for h in range(4):
                    for kc in range(2):
                        bk = 3 + kc
                        mmg(bk, 512, [(K2T[:, 2 * h + c, kc * 128:(kc + 1) * 128], q2[:, 2 * h + c, :]) for c in range(2)], ["K2T", "q2"])
                        op("act", lambda e, kc=kc, bk=bk: e.activation(out=P2[kc][:], in_=pb[bk][:], func=AF.Exp, scale=SC2), reads=[PB[bk]], writes=["P2_%d" % kc])
                    mmg(5, 512, [(onesb[:], P2[kc][:]) for kc in range(2)], ["onesb", "P2_0", "P2_1"])
                    op("dve", lambda e: e.reciprocal(out=rD2[:], in_=pb[5][:]), reads=[PB[5]], writes=["rD2"])
                    for dc in range(2):
                        bk = 6 + dc
                        mmg(bk, 512, [(V2[:, kc, h * 256 + dc * 128:h * 256 + (dc + 1) * 128], P2[kc][:]) for kc in range(2)], ["V2", "P2_0", "P2_1"])
                        op("dve", lambda e, h=h, dc=dc, bk=bk: e.tensor_tensor(out=o2[:, 2 * h + dc, :], in0=pb[bk][:], in1=rD2[:], op=ALU.mult), reads=[PB[bk], "rD2"], writes=["o2"])
                for m in range(8):
                    bk = 1 + (m % 2)
                    mmg(bk, 512, [(w2o[:, k, m * 128:(m + 1) * 128], o2[:, k, :]) for k in range(8)], ["w2o", "o2"])
                    op("dve", lambda e, m=m, tsl=tsl, bk=bk: e.tensor_tensor(out=x1[:, m, tsl], in0=pb[bk][:], in1=x1[:, m, tsl], op=ALU.add), reads=[PB[bk], "x1"], writes=["x1"])
            sch.barrier(skip=("zD",))
            sch.emit()

        pr = ExitStack()
        trib = sbt(pr, "trib", [128, 128], BF16)
        iot = sbt(pr, "iot", [128, 16], F32)
        iots = sbt(pr, "iots", [128, 1024], F32)
        myrows = sbt(pr, "myrows", [128, 4], I32)
        gsel = sbt(pr, "gsel", [128, 4, 16], F32)
        aff4 = sbt(pr, "aff4", [128, 4, 64], F32)
        lo = sbt(pr, "lo", [128, 4], F32)
        mid = sbt(pr, "mid", [128, 4], F32)
        cmp = sbt(pr, "cmp", [128, 4, 64], F32)
        csa = sbt(pr, "csa", [128, 4, 64], F32)
        cntb = sbt(pr, "cntb", [128, 4], BF16)
        cnt32 = sbt(pr, "cnt32", [128, 4], F32)
        ge = sbt(pr, "ge", [128, 4], F32)
        off32 = sbt(pr, "off32", [128, 4], F32)
        offa = sbt(pr, "offa", [128, 4], BF16)
        offa32 = sbt(pr, "offa32", [128, 4], F32)
        endp = sbt(pr, "endp", [128, 4], F32)
        rowsR = sbt(pr, "rowsR", [128, 4, 68], BF16)
        ohA = sbt(pr, "ohA", [128, 1024], F32)
        OH = sbt(pr, "OH", [128, 1024], BF16)
        offs = sbt(pr, "offs", [128, 8], F32)
        meta = sbt(pr, "meta", [128, 4, 4], F32)
        idxh = sbt(pr, "idxh", [128, 4, 8], I32)
        idxhf = sbt(pr, "idxhf", [128, 8], F32)
        jl = sbt(pr, "jl", [128, 8], F32)
        le = sbt(pr, "le", [128, 8, 64], F32)
        fi = sbt(pr, "fi", [128, 8], F32)
        idxf = sbt(pr, "idxf", [128, 8], F32)
        idx = sbt(pr, "idx", [128, 4, 8], I32)
        wg0 = sbt(pr, "wg0", [128, 8, D], BF16)
        wu0 = sbt(pr, "wu0", [128, 8, D], BF16)
        with ExitStack() as p4:
            wr = sbt(p4, "wr", [128, 8, 16], BF16)
            hsq4_ = [sbt(p4, "hsq4_%d" % i, [128, 8, 512], BF16) for i in range(2)]
            rs4_ = [sbt(p4, "rs4_%d" % i, [128, 512], F32) for i in range(2)]
            h3_ = [sbt(p4, "h3_%d" % i, [128, 8, 512], BF16) for i in range(4)]
            rows = [sbt(p4, "rows%d" % i, [128, 1024], BF16) for i in range(2)]
            aftm = [sbt(p4, "aftm%d" % i, [128, 4, 16], F32) for i in range(2)]
            mx4 = sbt(p4, "mx4", [128, 4], F32)
            sm4 = sbt(p4, "sm4", [128, 4], F32)
            sh4 = sbt(p4, "sh4", [128, 4, 16], F32)
            x2tm = [sbt(p4, "x2tm%d" % i, [128, D], F32) for i in range(2)]
            affT = sbt(p4, "affT", [16, TOK], F32)
            eT = sbt(p4, "eT", [16, 512], F32)
            rsT = sbt(p4, "rsT", [16, 512], F32)
            ones16 = sbt(p4, "ones16", [16, 16], F32)
            load_w(wr, w_router, "wr")
            dma("pool", lambda e: e.dma_start(out=trib[:], in_=tri_d), "c3", writes=["trib"])
            dma("sp", lambda e: e.dma_start(out=iot[:], in_=iot_d), "c4", writes=["iot"])
            dma("sp", lambda e: e.dma_start(out=iots[:], in_=iots_d), "c5", writes=["iots"])
            dma("sp", lambda e: e.dma_start(out=myrows[:], in_=myrows_d), "c6", writes=["myrows"])
            dma("sp", lambda e: e.dma_start(out=gsel[:], in_=gsel_d), "c7", writes=["gsel"])
            op("dve", lambda e: e.memset(ones16[:], 1.0), writes=["ones16"])
            ci = 0
            deferred_ag = []

            def hall_ag(a_):
                dma("pool", lambda e: e.collective_compute("AllGather", ALU.bypass, replica_groups=GROUPS, ins=[Hloc[a_ * 256:(a_ + 1) * 256, :]],
                                                           outs=[Hall[a_ * 1024:(a_ + 1) * 1024, :]]), "cc", reads=["Hloc_s%d" % (2 * a_), "Hloc_s%d" % (2 * a_ + 1)], writes=["Hall%d" % a_], inc=1)

            def start_routing_inputs():
                dma("sp", lambda e: e.dma_start(out=Aloc, in_=affT[:]), "st_aff", reads=["affT"], writes=["Aloc"])
                if os.environ.get("KSKIP_CC") == "1":
                    dma("pool", lambda e: e.dma_start(out=Aall[0:16, :], in_=Aloc), "cc", reads=["Aloc"], writes=["Aall"])
                else:
                    dma("pool", lambda e: e.collective_compute("AllGather", ALU.bypass, replica_groups=GROUPS, ins=[Aloc], outs=[Aall]), "cc2", reads=["Aloc"], writes=["Aall"], inc=1)
                Aall2 = Aall.rearrange("r (i f) -> (r i) f", f=64)
                for k in range(4):
                    dma("pool", lambda e, k=k: e.indirect_dma_start(out=aff4[:, k, :], out_offset=None, in_=Aall2,
                                                                   in_offset=bass.IndirectOffsetOnAxis(ap=myrows[:, k:k + 1], axis=0)),
                        "ga", reads=["Aall", "myrows"], writes=["aff4"])

            def norm3(t):
                tsl = slice(t * 512, (t + 1) * 512)
                hq, rs_, h_ = hsq4_[t % 2], rs4_[t % 2], h3_[t]
                kq, kr_, kh = "hsq4_%d" % (t % 2), "rs4_%d" % (t % 2), "h3_%d" % t
                op("act", lambda e: e.activation(out=hq[:], in_=x1[:, :, tsl], func=AF.Square), reads=["x1"], writes=[kq])
                rms_rstd(lambda k: hq[:, k, :], 8, 512, rs_[:], [kq], kr_, 1.0 / D, bank=0)
                for k in range(8):
                    op("dve", lambda e, k=k: e.scalar_tensor_tensor(out=h_[:, k, :], in0=x1[:, k, tsl], scalar=GMOE[:, k:k + 1], in1=rs_[:], op0=ALU.mult, op1=ALU.mult),
                       reads=["x1", kr_, "cst"], writes=[kh])
                mmg(1, 512, [(wr[:, k, :], h_[:, k, :]) for k in range(8)], ["wr", kh], m=16)
                op("act", lambda e: e.activation(out=eT[:], in_=pb[1][0:16, :], func=AF.Exp), reads=[PB[1]], writes=["eT"])
                op("pe", lambda e: e.matmul(pb[2][0:16, :], lhsT=ones16[:], rhs=eT[:], start=True, stop=True), reads=["ones16", "eT"], writes=[PB[2]])
                op("dve", lambda e: e.reciprocal(out=rsT[:], in_=pb[2][0:16, :]), reads=[PB[2]], writes=["rsT"])
                op("dve", lambda e: e.tensor_tensor(out=affT[:, tsl], in0=eT[:], in1=rsT[:], op=ALU.mult), reads=["eT", "rsT"], writes=["affT"])
                if t == 3:
                    start_routing_inputs()
            for t in range(4):
                norm3(t)
            for t in range(4):
                tsl = slice(t * 512, (t + 1) * 512)
                h3 = h3_[t]
                H3K = "h3_%d" % t
                for sc in range(4):
                    csl = slice(sc * 128, (sc + 1) * 128)
                    mmg(3, 16, [(h3[:, k, csl], wr[:, k, :]) for k in range(8)], ["wr", H3K], col0=sc * 16)
                lg = pb[3][:, 0:64].rearrange("p (s e) -> p s e", e=16)
                AT = aftm[t % 2]
                ak = "aftm%d" % (t % 2)
                op("dve", lambda e: e.reduce_max(out=mx4[:], in_=lg, axis=AX.X), reads=[PB[3]], writes=["mx4"])
                op("dve", lambda e: e.tensor_tensor(out=sh4[:], in0=lg, in1=bc(mx4[:], 16), op=ALU.subtract), reads=[PB[3], "mx4"], writes=["sh4"])
                op("act", lambda e: e.activation(out=sh4[:], in_=sh4[:], func=AF.Exp), reads=["sh4"], writes=["sh4"])
                op("dve", lambda e: e.reduce_sum(out=sm4[:], in_=sh4[:], axis=AX.X), reads=["sh4"], writes=["sm4"])
                op("dve", lambda e: e.reciprocal(out=sm4[:], in_=sm4[:]), reads=["sm4"], writes=["sm4"])
                op("dve", lambda e, AT=AT: e.tensor_tensor(out=AT[:], in0=sh4[:], in1=bc(sm4[:], 16), op=ALU.mult), reads=["sh4", "sm4"], writes=[ak])
                dma("sp", lambda e, AT=AT, t=t: e.dma_start(out=Gloc[t * 512:(t + 1) * 512, :].rearrange("(s p) e -> p s e", p=128), in_=AT[:]), "st_" + ak, reads=[ak], writes=["Gloc_" + ak])
                for sc in range(4):
                    rb = ci % 2
                    ci += 1
                    R_ = rows[rb]
                    rk = "rows%d" % rb
                    X_ = x2tm[rb]
                    xk = "x2tm%d" % rb
                    csl = slice(sc * 128, (sc + 1) * 128)
                    gsl = slice(t * 512 + sc * 128, t * 512 + (sc + 1) * 128)
                    brow = 4 if rb == 0 else 7
                    bx = (5, 6) if rb == 0 else (1, 2)
                    pbt = pb[brow].bitcast(BF16).rearrange("p (a b) -> p a b", b=128)
                    for k in range(8):
                        op("pe", lambda e, k=k, csl=csl, pbt=pbt, h3=h3: e.transpose(pbt[:, k, :], h3[:, k, csl], identb[:]), reads=[H3K, "identb"], writes=[PB[brow]], inc=(k == 7))
                    op("act", lambda e, R_=R_, brow=brow: e.copy(out=R_[:], in_=pb[brow].bitcast(BF16)), reads=[PB[brow]], writes=[rk])
                    dma("sp", lambda e, R_=R_, gsl=gsl: e.dma_start(out=Hloc[gsl, :], in_=R_[:]), "st_" + rk, reads=[rk], writes=["Hloc_s%d" % (ci - 1)])
                    if ci % 2 == 0 and os.environ.get("KSKIP_CC") != "1":
                        a_ = ci // 2 - 1
                        if a_ <= 7:
                            hall_ag(a_)
                        else:
                            deferred_ag.append(a_)
                    for k in range(8):
                        bk = bx[k // 4]
                        op("pe", lambda e, k=k, bk=bk, gsl=gsl: e.transpose(pb[bk][:, (k % 4) * 128:(k % 4 + 1) * 128], x1[:, k, gsl], identf[:]), reads=["x1", "identf"], writes=[PB[bk]], inc=(k % 4 == 3))
                    op("dve", lambda e, X_=X_, bx=bx: e.tensor_copy(out=X_[:, 0:512], in_=pb[bx[0]]), reads=[PB[bx[0]]], writes=[xk])
                    op("dve", lambda e, X_=X_, bx=bx: e.tensor_copy(out=X_[:, 512:1024], in_=pb[bx[1]]), reads=[PB[bx[1]]], writes=[xk])
                    dma("sp", lambda e, X_=X_, gsl=gsl: e.dma_start(out=X2loc[gsl, :], in_=X_[:]), "st_" + xk, reads=[xk], writes=["X2loc_" + xk])
            if os.environ.get("KSKIP_CC") != "1":
                for a_ in deferred_ag:
                    hall_ag(a_)
                dma("pool", lambda e: e.collective_compute("AllGather", ALU.bypass, replica_groups=GROUPS, ins=[Gloc], outs=[Gall]), "cc4", reads=["Gloc_aftm0", "Gloc_aftm1"], writes=["Gall"], inc=1)
            if debug:
                dma("sp", lambda e: e.dma_start(out=dbg["x2"], in_=X2loc), "dbg", reads=["X2loc_x2tm0", "X2loc_x2tm1"])
            load_w(wg0, w_eg[0], "wg0")
            load_w(wu0, w_eu[0], "wu0")
            op("dve", lambda e: e.memset(lo[:], 0.0), writes=["lo"])
            for it in range(32):
                step = float(2.0 ** -(it + 1))
                op("dve", lambda e, step=step: e.tensor_scalar(out=mid[:], in0=lo[:], scalar1=step, scalar2=None, op0=ALU.add), reads=["lo"], writes=["mid"])
                op("dve", lambda e: e.tensor_tensor(out=cmp[:], in0=aff4[:], in1=bc(mid[:], 64), op=ALU.is_ge), reads=["aff4", "mid"], writes=["cmp"])
                op("dve", lambda e: e.reduce_sum(out=cnt32[:], in_=cmp[:], axis=AX.X), reads=["cmp"], writes=["cnt32"])
                op("dve", lambda e: e.tensor_copy(out=cntb[:], in_=cnt32[:]), reads=["cnt32"], writes=["cntb"])
                op("pe", lambda e: e.matmul(pb[0][:, 0:4], lhsT=onesb[:], rhs=cntb[:], start=True, stop=True), reads=["onesb", "cntb"], writes=[PB[0]])
                op("dve", lambda e, step=step: e.tensor_scalar(out=ge[:], in0=pb[0][:, 0:4], scalar1=1023.5, scalar2=step, op0=ALU.is_ge, op1=ALU.mult), reads=[PB[0]], writes=["ge"])
                op("dve", lambda e: e.tensor_tensor(out=lo[:], in0=lo[:], in1=ge[:], op=ALU.add), reads=["lo", "ge"], writes=["lo"])
            op("dve", lambda e: e.tensor_tensor(out=cmp[:], in0=aff4[:], in1=bc(lo[:], 64), op=ALU.is_ge), reads=["aff4", "lo"], writes=["cmp"])
            op("dve", lambda e: e.reduce_sum(out=cnt32[:], in_=cmp[:], axis=AX.X), reads=["cmp"], writes=["cnt32"])
            op("dve", lambda e: e.tensor_copy(out=cntb[:], in_=cnt32[:]), reads=["cnt32"], writes=["cntb"])
            op("pe", lambda e: e.matmul(pb[0][:, 0:4], lhsT=trib[:], rhs=cntb[:], start=True, stop=True), reads=["trib", "cntb"], writes=[PB[0]])
            op("dve", lambda e: e.tensor_copy(out=off32[:], in_=pb[0][:, 0:4]), reads=[PB[0]], writes=["off32"])
            op("dve", lambda e: e.tensor_copy(out=offa[:], in_=off32[:]), reads=["off32"], writes=["offa"])
            op("dve", lambda e: e.tensor_copy(out=offa32[:], in_=offa[:]), reads=["offa"], writes=["offa32"])
            op("dve", lambda e: e.tensor_tensor(out=endp[:], in0=off32[:], in1=cnt32[:], op=ALU.add), reads=["off32", "cnt32"], writes=["endp"])
            src, dst, srck, dstk = cmp, csa, "cmp", "csa"
            for s_ in (1, 2, 4, 8, 16, 32):
                op("dve", lambda e, s_=s_, src=src, dst=dst: e.tensor_copy(out=dst[:, :, 0:s_], in_=src[:, :, 0:s_]), reads=[srck], writes=[dstk])
                op("dve", lambda e, s_=s_, src=src, dst=dst: e.tensor_tensor(out=dst[:, :, s_:64], in0=src[:, :, s_:64], in1=src[:, :, 0:64 - s_], op=ALU.add), reads=[srck], writes=[dstk])
                src, dst, srck, dstk = dst, src, dstk, srck
            csf, csk = src, srck
            op("dve", lambda e: e.tensor_copy(out=rowsR[:, :, 0:64], in_=csf[:]), reads=[csk], writes=["rowsR"])
            op("dve", lambda e: e.tensor_copy(out=rowsR[:, :, 64:65], in_=offa[:].unsqueeze(2)), reads=["offa"], writes=["rowsR"])
            op("dve", lambda e: e.tensor_tensor(out=rowsR[:, :, 65:66], in0=off32[:].unsqueeze(2), in1=offa32[:].unsqueeze(2), op=ALU.subtract), reads=["off32", "offa32"], writes=["rowsR"])
            op("dve", lambda e: e.tensor_copy(out=rowsR[:, :, 66:67], in_=iot[:, 8:9].unsqueeze(1).to_broadcast([128, 4, 1])), reads=["iot"], writes=["rowsR"])
            op("dve", lambda e: e.tensor_copy(out=rowsR[:, :, 67:68], in_=iot[:, 9:10].unsqueeze(1).to_broadcast([128, 4, 1])), reads=["iot"], writes=["rowsR"])
            if debug:
                dma("sp", lambda e: e.dma_start(out=dbg["thr"], in_=lo[:]), "dbg", reads=["lo"])
            pR = [pb[1], pb[2]]
            for k in range(4):
                op("dve", lambda e, k=k: e.tensor_scalar(out=ohA[:], in0=iots[:], scalar1=off32[:, k:k + 1], scalar2=None, op0=ALU.is_ge), reads=["iots", "off32"], writes=["ohA"])
                op("dve", lambda e, k=k: e.scalar_tensor_tensor(out=OH[:], in0=iots[:], scalar=endp[:, k:k + 1], in1=ohA[:], op0=ALU.is_lt, op1=ALU.mult), reads=["iots", "endp", "ohA"], writes=["OH"])
                for c in range(8):
                    bk = 1 + c // 4
                    op("pe", lambda e, c=c, k=k, bk=bk: e.matmul(pb[bk][:, (c % 4) * 128:(c % 4) * 128 + 68], lhsT=OH[:, c * 128:(c + 1) * 128], rhs=rowsR[:, k, :], start=True, stop=True),
                       reads=["OH", "rowsR"], writes=[PB[bk]])
                for hb in range(2):
                    pv = pb[1 + hb][:].rearrange("p (c w) -> p c w", w=128)
                    hs = slice(hb * 4, hb * 4 + 4)
                    op("dve", lambda e, pv=pv: e.tensor_copy(out=meta[:], in_=pv[:, :, 64:68]), reads=[PB[1 + hb]], writes=["meta"])
                    op("dve", lambda e, hs=hs: e.tensor_tensor(out=offs[:, hs].unsqueeze(2), in0=meta[:, :, 0:1], in1=meta[:, :, 1:2], op=ALU.add), reads=["meta"], writes=["offs"])
                    op("dve", lambda e, hs=hs: e.tensor_tensor(out=jl[:, hs], in0=iot[:, hs], in1=offs[:, hs], op=ALU.subtract), reads=["iot", "offs"], writes=["jl"])
                    op("dve", lambda e, pv=pv, hs=hs: e.tensor_tensor(out=le[:, hs, :], in0=pv[:, :, 0:64], in1=bc(jl[:, hs], 64), op=ALU.is_le), reads=[PB[1 + hb], "jl"], writes=["le"])
                    op("dve", lambda e, hs=hs: e.reduce_sum(out=fi[:, hs], in_=le[:, hs, :], axis=AX.X), reads=["le"], writes=["fi"])
                    op("dve", lambda e, hs=hs: e.scalar_tensor_tensor(out=idxf[:, hs].unsqueeze(2), in0=meta[:, :, 2:3], scalar=64.0, in1=fi[:, hs].unsqueeze(2), op0=ALU.mult, op1=ALU.add),
                       reads=["meta", "fi"], writes=["idxf"])
                    op("dve", lambda e, hs=hs: e.scalar_tensor_tensor(out=idxhf[:, hs].unsqueeze(2), in0=meta[:, :, 3:4], scalar=64.0, in1=fi[:, hs].unsqueeze(2), op0=ALU.mult, op1=ALU.add),
                       reads=["meta", "fi"], writes=["idxhf"])
                op("dve", lambda e, k=k: e.tensor_copy(out=idx[:, k, :], in_=idxf[:]), reads=["idxf"], writes=["idx"])
                op("dve", lambda e, k=k: e.tensor_copy(out=idxh[:, k, :], in_=idxhf[:]), reads=["idxhf"], writes=["idxh"])
            if debug:
                dma("sp", lambda e: e.dma_start(out=dbg["idx"], in_=idx[:].rearrange("p a b -> p (a b)")), "dbg", reads=["idx"])
            sch.barrier()
            sch.emit()

        with ExitStack() as p5:
            xrow = [sbt(p5, "xrow%d" % i, [128, 1024], BF16) for i in range(8)]
            gate = sbt(p5, "gate", [128, 4, 8], F32)
            gt4 = sbt(p5, "gt4", [128, 4, 8, 16], F32)
            gtm = sbt(p5, "gtm", [128, 8, 16], F32)
            xgT = sbt(p5, "xgT", [128, 8, 1024], BF16)
            aT = sbt(p5, "aT", [128, 8, 1024], BF16)
            wg = [wg0, sbt(p5, "wg1", [128, 8, D], BF16)]
            wu = [wu0, sbt(p5, "wu1", [128, 8, D], BF16)]
            wd = [sbt(p5, "wd%d" % i, [128, 8, D], BF16) for i in range(2)]
            sg = sbt(p5, "sg", [128, 512], F32)
            yrow = [sbt(p5, "yrow%d" % i, [128, D], F32) for i in range(2)]

            def load_exp(k):
                load_w(wg[k % 2], w_eg[k], "wg%d" % (k % 2))
                load_w(wu[k % 2], w_eu[k], "wu%d" % (k % 2))
                load_w(wd[k % 2], w_ed[k], "wd%d" % (k % 2))

            load_w(wd[0], w_ed[0], "wd0")

            yi = 0

            def gather_expert(k):
                for c in range(8):
                    XR = xrow[c]
                    xk = "xrow%d" % c
                    dma("pool", lambda e, XR=XR, k=k, c=c: e.indirect_dma_start(out=XR[:], out_offset=None, in_=Hall,
                                                                               in_offset=bass.IndirectOffsetOnAxis(ap=idxh[:, k, c:c + 1], axis=0)),
                        "g_" + xk, reads=["Hall", "idxh"], writes=[xk])

            def transpose_expert(k):
                for c in range(8):
                    XR = xrow[c]
                    xk = "xrow%d" % c
                    pbt = pb[3 + (c % 2)].bitcast(BF16).rearrange("p (a b) -> p a b", b=128)
                    for dk in range(8):
                        op("pe", lambda e, dk=dk, XR=XR, pbt=pbt: e.transpose(pbt[:, dk, :], XR[:, dk * 128:(dk + 1) * 128], identb[:]), reads=[xk, "identb"], writes=[PB[3 + (c % 2)]], inc=(dk == 7))
                    op("act", lambda e, c=c, pbt=pbt: e.copy(out=xgT[:, :, c * 128:(c + 1) * 128], in_=pbt[:, 0:8, :]), reads=[PB[3 + (c % 2)]], writes=["xgT"])

            gather_expert(0)
            for k in range(4):
                for c in range(8):
                    dma("pool", lambda e, k=k, c=c: e.indirect_dma_start(out=gt4[:, k, c, :], out_offset=None, in_=Gall,
                                                                        in_offset=bass.IndirectOffsetOnAxis(ap=idx[:, k, c:c + 1], axis=0)),
                        "g_gt%d" % k, reads=["Gall", "idx"], writes=["gt4_%d" % k])
            transpose_expert(0)
            for k in range(4):
                if k + 1 < 4:
                    load_exp(k + 1)
                WG, WU, WD = wg[k % 2], wu[k % 2], wd[k % 2]
                kg, ku, kd = "wg%d" % (k % 2), "wu%d" % (k % 2), "wd%d" % (k % 2)
                op("dve", lambda e, k=k: e.tensor_tensor(out=gtm[:], in0=gt4[:, k, :, :], in1=gsel[:, k, :].unsqueeze(1).to_broadcast([128, 8, 16]), op=ALU.mult),
                   reads=["gt4_%d" % k, "gsel"], writes=["gtm"])
                op("dve", lambda e, k=k: e.reduce_sum(out=gate[:, k, :], in_=gtm[:], axis=AX.X), reads=["gtm"], writes=["gate"])
                for st in range(2):
                    ssl = slice(st * 512, (st + 1) * 512)
                    for fc in range(8):
                        fsl = slice(fc * 128, (fc + 1) * 128)
                        bg_, bu_ = (5, 6) if fc % 2 == 0 else (7, 0)
                        mmg(bg_, 512, [(WG[:, dk, fsl], xgT[:, dk, ssl]) for dk in range(8)], [kg, "xgT"])
                        mmg(bu_, 512, [(WU[:, dk, fsl], xgT[:, dk, ssl]) for dk in range(8)], [ku, "xgT"])
                        op("act", lambda e, bg_=bg_: e.activation(out=sg[:], in_=pb[bg_][:], func=AF.Silu), reads=[PB[bg_]], writes=["sg"])
                        op("dve", lambda e, bu_=bu_, fc=fc, ssl=ssl: e.tensor_tensor(out=aT[:, fc, ssl], in0=pb[bu_][:], in1=sg[:], op=ALU.mult), reads=[PB[bu_], "sg"], writes=["aT"])
                if k + 1 < 4:
                    gather_expert(k + 1)
                for c in range(8):
                    yb = yi % 2
                    yi += 1
                    YR = yrow[yb]
                    yk = "yrow%d" % yb
                    csl = slice(c * 128, (c + 1) * 128)
                    for nh in range(2):
                        bk = 1 + nh
                        mmg(bk, 512, [(aT[:, fc, csl], WD[:, fc, nh * 512:(nh + 1) * 512]) for fc in range(8)], [kd, "aT"])
                        op("dve" if nh == 0 else "act",
                           (lambda e, YR=YR, k=k, c=c, bk=bk, nh=nh: e.tensor_scalar(out=YR[:, nh * 512:(nh + 1) * 512], in0=pb[bk][:], scalar1=gate[:, k, c:c + 1], scalar2=None, op0=ALU.mult))
                           if nh == 0 else
                           (lambda e, YR=YR, k=k, c=c, bk=bk, nh=nh: e.mul(out=YR[:, nh * 512:(nh + 1) * 512], in_=pb[bk][:], mul=gate[:, k, c:c + 1])),
                           reads=[PB[bk], "gate"], writes=[yk])
                    dma("pool", lambda e, YR=YR, k=k, c=c: e.indirect_dma_start(out=Dd, out_offset=bass.IndirectOffsetOnAxis(ap=idx[:, k, c:c + 1], axis=0),
                                                                               in_=YR[:], in_offset=None, compute_op=ALU.add),
                        "sc", reads=[yk, "idx", "Dd"], writes=["Dd"])
                if k + 1 < 4:
                    transpose_expert(k + 1)
            if os.environ.get("KSKIP_CC") == "1":
                dma("pool", lambda e: e.dma_start(out=Rr, in_=Dd[0:TOK, :]), "cc", reads=["Dd"], writes=["Rr"])
            else:
                dma("pool", lambda e: e.collective_compute("ReduceScatter", ALU.add, replica_groups=GROUPS, ins=[Dd], outs=[Rr]), "cc3", reads=["Dd"], writes=["Rr"], inc=1)
            if debug:
                dma("sp", lambda e: e.dma_start(out=dbg["R"], in_=Rr), "dbg", reads=["Rr"])
            sch.barrier()
            sch.emit()

        pr.close()
        with ExitStack() as p6:
            gfin = sbt(p6, "gfin", [128, D], F32)
            xr = [sbt(p6, "xr%d" % i, [128, D], F32) for i in range(2)]
            rr = [sbt(p6, "rr%d" % i, [128, D], F32) for i in range(2)]
            zz = [sbt(p6, "zz%d" % i, [128, D], F32) for i in range(2)]
            oo = [sbt(p6, "oo%d" % i, [128, D], F32) for i in range(2)]
            sqj = sbt(p6, "sqj", [128, D], F32)
            ssum = sbt(p6, "ssum", [128, 1], F32)
            dma("sp", lambda e: e.dma_start(out=gfin[:], in_=gfin_d), "c8", writes=["gfin"])
            sqj2 = [sqj, sbt(p6, "sqjb", [128, D], F32)]
            ssum2 = [ssum, sbt(p6, "ssumb", [128, 1], F32)]

            def ld(c):
                b = c % 2
                gsl = slice(c * 128, (c + 1) * 128)
                dma("sp", lambda e, b=b, gsl=gsl: e.dma_start(out=xr[b][:], in_=X2loc[gsl, :]), "ldx%d" % b, reads=["X2loc"], writes=["xr%d" % b])
                dma("sp", lambda e, b=b, gsl=gsl: e.dma_start(out=rr[b][:], in_=Rr[gsl, :]), "ldr%d" % b, reads=["Rr"], writes=["rr%d" % b])
            ld(0)
            for c in range(16):
                b = c % 2
                gsl = slice(c * 128, (c + 1) * 128)
                op("dve", lambda e, b=b: e.tensor_tensor(out=zz[b][:], in0=xr[b][:], in1=rr[b][:], op=ALU.add), reads=["xr%d" % b, "rr%d" % b], writes=["zz%d" % b])
                if c + 1 < 16:
                    ld(c + 1)
                op("act", lambda e, b=b: e.activation(out=sqj2[b][:], in_=zz[b][:], func=AF.Square), reads=["zz%d" % b], writes=["sqj%d" % b])
                op("dve", lambda e, b=b: e.reduce_sum(out=ssum2[b][:], in_=sqj2[b][:], axis=AX.X), reads=["sqj%d" % b], writes=["ssum%d" % b])
                op("act", lambda e, b=b: e.activation(out=ssum2[b][:], in_=ssum2[b][:], func=AF.Sqrt, bias=EPS, scale=1.0 / D), reads=["ssum%d" % b], writes=["ssum%d" % b])
                op("dve", lambda e, b=b: e.reciprocal(out=ssum2[b][:], in_=ssum2[b][:]), reads=["ssum%d" % b], writes=["ssum%d" % b])
                op("dve", lambda e, b=b: e.scalar_tensor_tensor(out=oo[b][:], in0=zz[b][:], scalar=ssum2[b][:, 0:1], in1=gfin[:], op0=ALU.mult, op1=ALU.mult),
                   reads=["zz%d" % b, "ssum%d" % b, "gfin"], writes=["oo%d" % b])
                dma("act", lambda e, b=b, gsl=gsl: e.dma_start(out=out[gsl, :], in_=oo[b][:]), "sto%d" % b, reads=["oo%d" % b])
            sch.barrier()
            sch.emit()
    return nc


def rope_tables(pos0, n):
    inv = 1.0 / (10000.0 ** (np.arange(0, 64, 2, dtype=np.float32) / 64.0))
    ang = (np.arange(pos0, pos0 + n, dtype=np.float32)[:, None] * inv[None, :].astype(np.float32)).astype(np.float32)
    cos = np.cos(ang).astype(np.float32).T
    sin = np.sin(ang).astype(np.float32).T
    cos2 = np.concatenate([cos, cos], 0)
    sin2 = np.concatenate([-sin, sin], 0)
    return np.ascontiguousarray(np.stack([cos2, sin2], 1)).astype(np.float32)


def make_inputs(x, mem, norm_mix_g, w_in, conv_w, conv_b, w_conv_out, q_norm_g, w_uq, kv_norm_g,
                w_ukv, w_mla_out, b_gate, w_mix_out, norm_mem_g, norm_memkv_g, w_mem_q, w_mem_kv,
                w_mem_out, norm_moe_g, w_router, w_exp_gate, w_exp_up, w_exp_down, norm_final_g):
    f = lambda a: np.ascontiguousarray(np.asarray(a, dtype=np.float32))
    x = f(x); mem = f(mem)
    w_in0 = f(w_in[0])
    col = lambda vec: np.ascontiguousarray(f(vec).reshape(-1, 128).T)
    cst = np.concatenate([col(norm_mix_g[0]), col(conv_w[0][0]), col(conv_w[0][1]), col(conv_w[0][2]), col(conv_b[0]),
                          col(q_norm_g[0]), col(kv_norm_g[0]), col(b_gate[0]), col(norm_mem_g[0]), col(norm_memkv_g[0]),
                          col(norm_moe_g[0])], axis=1)
    assert cst.shape == (128, NCST)
    perm = np.concatenate([np.arange(32, 64), np.arange(0, 32)])
    w_krsw = np.ascontiguousarray(w_in0[:, 3456:3520][:, perm])
    wuq = f(w_uq[0])
    w_uqs = np.ascontiguousarray(np.concatenate([wuq[:, h * 192 + 128:h * 192 + 192][:, perm] for h in range(8)], axis=1))
    wukv = f(w_ukv[0])
    w_ukT = np.ascontiguousarray(np.stack([wukv[:, h * 256:h * 256 + 128].T for h in range(8)], axis=1))
    ident = np.eye(128, dtype=np.float32)
    tri = np.triu(np.ones((128, 128), np.float32), 1)
    iot = np.zeros((128, 16), np.float32)
    for c in range(8):
        iot[:, c] = c * 128 + np.arange(128)
    iot[:, 8] = np.arange(128)
    pp = np.arange(128)
    iot[:, 9] = ((pp % 32) // 4) * 16 + (pp // 32) * 4 + (pp % 4)
    iots = np.ascontiguousarray(np.broadcast_to(np.arange(1024, dtype=np.float32)[None, :], (128, 1024)))
    gfin = np.ascontiguousarray(np.broadcast_to(f(norm_final_g)[None, :], (128, D)))
    ropeb = rope_tables(0, S)
    shared = dict(w_in=w_in0, w_krsw=w_krsw, w_conv_out=f(w_conv_out[0]), w_uq=wuq, w_uqs=w_uqs, w_ukT=w_ukT, w_ukv=wukv,
                  w_mla_out=f(w_mla_out[0]), w_mix_out=f(w_mix_out[0]), w_mem_q=f(w_mem_q[0]), w_mem_kv=f(w_mem_kv[0]),
                  w_mem_out=f(w_mem_out[0]), w_router=f(w_router[0]), cst=cst, gfin=gfin, ident=ident, tri=tri, iot=iot,
                  iots=iots, ropeb=ropeb)
    xT = [np.ascontiguousarray(x[b].T) for b in range(2)]
    memT = [np.ascontiguousarray(mem[b].T) for b in range(2)]
    weg, weu, wed = f(w_exp_gate[0]), f(w_exp_up[0]), f(w_exp_down[0])
    in_maps = []
    for c in range(8):
        b, r = c // 4, c % 4
        s0 = r * TOK
        xo = np.zeros((D, TOK + 2), np.float32)
        lo_, hi_ = max(s0 - 1, 0), min(s0 + TOK + 1, S)
        xo[:, lo_ - (s0 - 1):hi_ - (s0 - 1)] = xT[b][:, lo_:hi_]
        p = np.arange(128)
        myrows = np.stack([((p // 32) * 16 + 4 * r + k) * 32 + (p % 32) for k in range(4)], axis=1).astype(np.int32)
        gsel = np.zeros((128, 4, 16), np.float32)
        for k in range(4):
            gsel[:, k, 4 * r + k] = 1.0
        m = dict(shared)
        m.update(xTb=xT[b], xown=xo, memT=memT[b], ropeo=np.ascontiguousarray(ropeb[:, :, s0:s0 + TOK]),
                 w_eg=np.ascontiguousarray(weg[4 * r:4 * r + 4]), w_eu=np.ascontiguousarray(weu[4 * r:4 * r + 4]),
                 w_ed=np.ascontiguousarray(wed[4 * r:4 * r + 4]), myrows=myrows, gsel=gsel)
        in_maps.append(m)
    return in_maps


_NC_CACHE = {}


def run(inputs, debug=False):
    if debug not in _NC_CACHE:
        _NC_CACHE[debug] = build(debug)
    nc = _NC_CACHE[debug]
    in_maps = make_inputs(**inputs)
    res = run_bass_kernel_spmd(nc, in_maps, core_ids=list(range(8)))
    return res


def kernel(**inputs):
    res = run(inputs, debug=False)
    outs = [np.asarray(res.results[c]["out"], dtype=np.float32) for c in range(8)]
    full = np.stack([np.concatenate(outs[0:4], axis=0), np.concatenate(outs[4:8], axis=0)], axis=0)
    return full.astype(np.float32)
```
